# Optimizing a Trainium2 kernel written in Bass

```python
import jax, jax.numpy as jnp
from jax import lax
import numpy as np

D_MODEL = 1024
BATCH = 32
SEQ = 2048
DEPTH = 1

CHUNK = 64
Q_BLOCK = 128
HEAD_DIM = 64
N_HEADS_FOX = 8
N_HEADS_DSA = 8
IDX_HEADS = 8
IDX_DIM = 64
TOPK_MAX = 256
ROPE_THETA = 10000.0
N_GROUPS = 4
EXPERTS_PER_GROUP = 8
N_EXPERTS = N_GROUPS * EXPERTS_PER_GROUP
TOP_K_INNER = 2
D_EXPERT = 512
EPS = 1e-6

W_FOX = N_HEADS_FOX * HEAD_DIM
W_DSA = N_HEADS_DSA * HEAD_DIM
IN_SPLITS = (W_FOX, W_FOX, W_FOX, N_HEADS_FOX,
             W_DSA, HEAD_DIM, HEAD_DIM,
             IDX_HEADS * IDX_DIM, IDX_DIM, IDX_HEADS,
             D_MODEL, D_MODEL)
IN_COLS = 3 * W_FOX + N_HEADS_FOX + W_DSA + 2 * HEAD_DIM + IDX_HEADS * IDX_DIM + IDX_DIM + IDX_HEADS + 2 * D_MODEL

kernel_name = 'streaming_hybrid_fox_dsa_hmoe_block'


def rmsnorm(x, g):
    xf = x.astype(jnp.float32)
    y = xf * lax.rsqrt(jnp.mean(xf * xf, axis=-1, keepdims=True) + EPS)
    return (y * g.astype(jnp.float32)).astype(x.dtype)


def rope(x, pos):
    half = x.shape[-1] // 2
    inv = ROPE_THETA ** (-jnp.arange(half, dtype=jnp.float32) / half)
    ang = pos.astype(jnp.float32)[..., None] * inv
    cos = jnp.cos(ang)[:, :, None, :]
    sin = jnp.sin(ang)[:, :, None, :]
    x1 = x[..., :half].astype(jnp.float32)
    x2 = x[..., half:].astype(jnp.float32)
    out = jnp.concatenate([x1 * cos - x2 * sin, x2 * cos + x1 * sin], axis=-1)
    return out.astype(x.dtype)


def forgetting_attention(q, k, v, log_f):
    B, S, H, D = q.shape
    Ft = jnp.cumsum(log_f, axis=1).transpose(0, 2, 1)
    scale = D ** -0.5
    outs = []
    for start in range(0, S, Q_BLOCK):
        end = start + Q_BLOCK
        s = jnp.einsum('bqhd,bkhd->bhqk', q[:, start:end], k[:, :end],
                       preferred_element_type=jnp.float32) * scale
        s = s + Ft[:, :, start:end, None] - Ft[:, :, None, :end]
        tq = jnp.arange(start, end)[:, None]
        tk = jnp.arange(end)[None, :]
        s = jnp.where(tk <= tq, s, -jnp.inf)
        p = jax.nn.softmax(s, axis=-1)
        outs.append(jnp.einsum('bhqk,bkhd->bqhd', p.astype(v.dtype), v[:, :end]))
    return jnp.concatenate(outs, axis=1)


def dsa_attention(q, k, v, qi, ki, wi):
    B, S, H, D = q.shape
    topk = min(TOPK_MAX, S // 4)
    chunk_id = jnp.arange(S) // CHUNK
    scale = D ** -0.5
    gather = jax.vmap(lambda arr, ids: arr[ids])
    outs = []
    for start in range(0, S, Q_BLOCK):
        end = start + Q_BLOCK
        kk = min(topk, end)
        dots = jnp.einsum('bqhd,bkd->bqhk', qi[:, start:end], ki[:, :end],
                          preferred_element_type=jnp.float32)
        score = jnp.einsum('bqh,bqhk->bqk', wi[:, start:end].astype(jnp.float32), jax.nn.relu(dots))
        cq = chunk_id[start:end]
        allowed = chunk_id[:end][None, :] <= cq[:, None]
        score = jnp.where(allowed[None], score, -jnp.inf)
        _, idx = lax.top_k(score, kk)
        valid = chunk_id[idx] <= cq[None, :, None]
        kg = gather(k[:, :end], idx)
        vg = gather(v[:, :end], idx)
        s = jnp.einsum('bqhd,bqkd->bhqk', q[:, start:end], kg,
                       preferred_element_type=jnp.float32) * scale
        s = jnp.where(valid[:, None], s, -jnp.inf)
        p = jax.nn.softmax(s, axis=-1)
        outs.append(jnp.einsum('bhqk,bqkd->bqhd', p.astype(vg.dtype), vg))
    return jnp.concatenate(outs, axis=1)


def hierarchical_moe(h, w_grp, b_grp, w_exp, b_exp, w1, w3, w2):
    N, D = h.shape
    g_logits = jnp.matmul(h, w_grp).astype(jnp.float32) + b_grp.astype(jnp.float32)
    g_prob = jax.nn.softmax(g_logits, axis=-1)
    g_idx = jnp.argmax(g_logits, axis=-1)
    g_w = jnp.take_along_axis(g_prob, g_idx[:, None], axis=1)[:, 0]
    e_logits = (jnp.matmul(h, w_exp).astype(jnp.float32) + b_exp.astype(jnp.float32))
    e_logits = e_logits.reshape(N, N_GROUPS, EXPERTS_PER_GROUP)
    e_logits = jnp.take_along_axis(e_logits, g_idx[:, None, None], axis=1)[:, 0]
    e_prob = jax.nn.softmax(e_logits, axis=-1)
    top_p, top_local = lax.top_k(e_prob, TOP_K_INNER)
    top_p = top_p / jnp.sum(top_p, axis=-1, keepdims=True)
    weights = (g_w[:, None] * top_p).reshape(-1)
    expert = (g_idx[:, None] * EXPERTS_PER_GROUP + top_local).reshape(-1)
    order = jnp.argsort(expert)
    tok = order // TOP_K_INNER
    xs = h[tok]
    sizes = jnp.bincount(expert, length=N_EXPERTS).astype(jnp.int32)
    a = lax.ragged_dot(xs, w1, sizes)
    b = lax.ragged_dot(xs, w3, sizes)
    y = lax.ragged_dot(jax.nn.silu(a) * b, w2, sizes)
    y = (y * weights[order][:, None]).astype(h.dtype)
    return jax.ops.segment_sum(y, tok, num_segments=N)


def setup_inputs(seed: int = 0) -> dict:
    key = jax.random.key(seed)
    ks = jax.random.split(key, 24)
    f32 = jnp.float32
    nrm = lambda k, shape, fan: jax.random.normal(k, shape, f32) * fan ** -0.5
    L = DEPTH
    x = jax.random.normal(ks[0], (BATCH, SEQ, D_MODEL), f32)
    c = jax.random.normal(ks[1], (BATCH, D_MODEL), f32)
    offsets = jax.random.randint(ks[2], (BATCH,), 0, 64) * CHUNK
    positions = (offsets[:, None] + jnp.arange(SEQ)[None, :]).astype(jnp.int32)
    return {
        'x': x,
        'c': c,
        'positions': positions,
        'ada_w': nrm(ks[3], (L, D_MODEL, 6 * D_MODEL), D_MODEL) * 0.5,
        'ada_b': jax.random.normal(ks[4], (L, 6 * D_MODEL), f32) * 0.02,
        'norm1_g': 1.0 + 0.05 * jax.random.normal(ks[5], (L, D_MODEL), f32),
        'norm2_g': 1.0 + 0.05 * jax.random.normal(ks[6], (L, D_MODEL), f32),
        'w_in': nrm(ks[7], (L, D_MODEL, IN_COLS), D_MODEL),
        'b_fgt': jax.random.uniform(ks[8], (L, N_HEADS_FOX), f32, 1.0, 4.0),
        'b_gate': jax.random.normal(ks[9], (L, 2 * D_MODEL), f32) * 0.02,
        'qn_fox': 1.0 + 0.05 * jax.random.normal(ks[10], (L, HEAD_DIM), f32),
        'kn_fox': 1.0 + 0.05 * jax.random.normal(ks[11], (L, HEAD_DIM), f32),
        'qn_dsa': 1.0 + 0.05 * jax.random.normal(ks[12], (L, HEAD_DIM), f32),
        'kn_dsa': 1.0 + 0.05 * jax.random.normal(ks[13], (L, HEAD_DIM), f32),
        'w_proj_fox': nrm(ks[14], (L, W_FOX, D_MODEL), W_FOX),
        'w_proj_dsa': nrm(ks[15], (L, W_DSA, D_MODEL), W_DSA),
        'w_out': nrm(ks[16], (L, D_MODEL, D_MODEL), D_MODEL),
        'router_w_grp': nrm(ks[17], (L, D_MODEL, N_GROUPS), D_MODEL),
        'router_b_grp': jax.random.normal(ks[18], (L, N_GROUPS), f32) * 0.01,
        'router_w_exp': nrm(ks[19], (L, D_MODEL, N_EXPERTS), D_MODEL),
        'router_b_exp': jax.random.normal(ks[20], (L, N_EXPERTS), f32) * 0.01,
        'exp_w1': nrm(ks[21], (L, N_EXPERTS, D_MODEL, D_EXPERT), D_MODEL),
        'exp_w3': nrm(ks[22], (L, N_EXPERTS, D_MODEL, D_EXPERT), D_MODEL),
        'exp_w2': nrm(ks[23], (L, N_EXPERTS, D_EXPERT, D_MODEL), D_EXPERT),
    }


def reference(x, c, positions, ada_w, ada_b, norm1_g, norm2_g, w_in, b_fgt, b_gate,
              qn_fox, kn_fox, qn_dsa, kn_dsa, w_proj_fox, w_proj_dsa, w_out,
              router_w_grp, router_b_grp, router_w_exp, router_b_exp,
              exp_w1, exp_w3, exp_w2):
    B, S, D = x.shape
    split_at = np.cumsum(IN_SPLITS)[:-1].tolist()
    c_act = jax.nn.silu(c)
    for l in range(DEPTH):
        mod = jnp.matmul(c_act, ada_w[l]) + ada_b[l]
        sh1, sc1, gt1, sh2, sc2, gt2 = [m[:, None, :] for m in jnp.split(mod, 6, axis=-1)]

        h = rmsnorm(x, norm1_g[l]) * (1.0 + sc1) + sh1
        proj = jnp.matmul(h, w_in[l])
        (fq, fk, fv, flog, dq, dk, dv, iq, ik, iw, g_fox, g_dsa) = jnp.split(proj, split_at, axis=-1)

        fq = rmsnorm(fq.reshape(B, S, N_HEADS_FOX, HEAD_DIM), qn_fox[l])
        fk = rmsnorm(fk.reshape(B, S, N_HEADS_FOX, HEAD_DIM), kn_fox[l])
        fv = fv.reshape(B, S, N_HEADS_FOX, HEAD_DIM)
        log_f = jax.nn.log_sigmoid((flog + b_fgt[l]).astype(jnp.float32))
        out_fox = forgetting_attention(fq, fk, fv, log_f).reshape(B, S, W_FOX)

        dq = rope(rmsnorm(dq.reshape(B, S, N_HEADS_DSA, HEAD_DIM), qn_dsa[l]), positions)
        dk = rope(rmsnorm(dk.reshape(B, S, 1, HEAD_DIM), kn_dsa[l]), positions)[:, :, 0]
        iq = rope(iq.reshape(B, S, IDX_HEADS, IDX_DIM), positions)
        ik = rope(ik.reshape(B, S, 1, IDX_DIM), positions)[:, :, 0]
        out_dsa = dsa_attention(dq, dk, dv, iq, ik, iw).reshape(B, S, W_DSA)

        merged = (jax.nn.sigmoid(g_fox + b_gate[l, :D]) * jnp.matmul(out_fox, w_proj_fox[l])
                  + jax.nn.sigmoid(g_dsa + b_gate[l, D:]) * jnp.matmul(out_dsa, w_proj_dsa[l]))
        x = x + gt1 * jnp.matmul(merged, w_out[l])

        h2 = rmsnorm(x, norm2_g[l]) * (1.0 + sc2) + sh2
        y = hierarchical_moe(h2.reshape(B * S, D), router_w_grp[l], router_b_grp[l],
                             router_w_exp[l], router_b_exp[l],
                             exp_w1[l], exp_w3[l], exp_w2[l]).reshape(B, S, D)
        x = x + gt2 * y
    return x
```

```python
import numpy as np
from contextlib import ExitStack
import concourse.bass as bass
import concourse.mybir as mybir
from concourse.bass_utils import run_bass_kernel_spmd

F32 = mybir.dt.float32
BF16 = mybir.dt.bfloat16
I32 = mybir.dt.int32
ALU = mybir.AluOpType
AF = mybir.ActivationFunctionType
AX = mybir.AxisListType

S = 2048
D = 1024
NT = 16
NCH = 4
KC = 8
NE = 32
C_FQ, C_FK, C_FV, C_FL, C_DQ, C_DK, C_DV, C_IQ, C_IK, C_IW, C_GF, C_GD = (
    0, 512, 1024, 1536, 1544, 2056, 2120, 2184, 2696, 2760, 2768, 3792)
IN_COLS = 4816
EPS = 1e-6
NEG = -30000.0
N_BISECT = 22
TWO_PI = 6.283185307179586


class T:
    __slots__ = ("w", "r")

    def __init__(self):
        self.w = {}
        self.r = {}


class KB:
    def __init__(self, nc, es, n_sp=12, n_pool=6):
        self.nc = nc
        self.E = {"pe": nc.tensor, "act": nc.scalar, "dve": nc.vector, "pool": nc.gpsimd, "sp": nc.sync}
        self.semobj = {}
        self.cnt = {}
        for e in ("pe", "act", "dve", "pool"):
            self.semobj[e] = es.enter_context(nc.semaphore("s_" + e))
            self.cnt[e] = 0
        self.known = {e: {} for e in self.E}
        self.dsems = {"sp": [], "pool": []}
        self.dtot = {}
        self.dnext = {"sp": 0, "pool": 0}
        for q, n in (("sp", n_sp), ("pool", n_pool)):
            for i in range(n):
                name = "d_%s%d" % (q, i)
                self.semobj[name] = es.enter_context(nc.semaphore(name))
                self.dsems[q].append(name)
                self.dtot[name] = 0

    @staticmethod
    def _deps(R, W):
        deps = {}
        for t in R:
            for k, v in t.w.items():
                if deps.get(k, 0) < v:
                    deps[k] = v
        for t in W:
            for d in (t.w, t.r):
                for k, v in d.items():
                    if deps.get(k, 0) < v:
                        deps[k] = v
        return deps

    def _wait(self, eng, deps):
        kn = self.known[eng]
        for k, v in deps.items():
            if eng == "pe" and k == "pe":
                continue
            if kn.get(k, 0) >= v:
                continue
            self.E[eng].wait_ge(self.semobj[k], v)
            kn[k] = v

    def op(self, eng, fn, R=(), W=()):
        self._wait(eng, self._deps(R, W))
        ins = fn(self.E[eng])
        self.cnt[eng] += 1
        c = self.cnt[eng]
        ins.then_inc(self.semobj[eng], 1)
        for t in W:
            t.w = {eng: c}
            t.r = {}
        for t in R:
            if t.r.get(eng, 0) < c:
                t.r[eng] = c

    def dma(self, q, out, in_, R=(), W=()):
        deps = self._deps(R, W)
        i = self.dnext[q]
        self.dnext[q] = (i + 1) % len(self.dsems[q])
        name = self.dsems[q][i]
        tot = self.dtot[name]
        if deps.get(name, 0) < tot:
            deps[name] = tot
        self._wait(q, deps)
        self.E[q].dma_start(out=out, in_=in_).then_inc(self.semobj[name], 16)
        tot += 16
        self.dtot[name] = tot
        for t in W:
            t.w = {name: tot}
            t.r = {}
        for t in R:
            t.r[name] = tot

    def barrier(self, engs=("pe", "act", "dve", "pool", "sp")):
        allv = dict(self.cnt)
        allv.update(self.dtot)
        allv = {k: v for k, v in allv.items() if v > 0}
        for e in engs:
            self._wait(e, dict(allv))

    def mm(self, out, lhsT, rhs, start, stop, R, W):
        self.op("pe", lambda e: e.matmul(out, lhsT, rhs, start=start, stop=stop), R, W)

    def tr(self, out, in_, ident, R, W):
        self.op("pe", lambda e: e.transpose(out, in_, ident), R, W)

    def act(self, out, in_, func, R, W, bias=0.0, scale=1.0, accum=None):
        if accum is None:
            self.op("act", lambda e: e.activation(out, in_, func, bias=bias, scale=scale), R, W)
        else:
            self.op("act", lambda e: e.activation(out, in_, func, bias=bias, scale=scale, accum_out=accum), R, W)

    def ts(self, eng, out, in0, s1, s2, op0, op1, R, W, accum=None):
        if accum is None:
            if op1 is None:
                self.op(eng, lambda e: e.tensor_scalar(out, in0, s1, None, op0), R, W)
            else:
                self.op(eng, lambda e: e.tensor_scalar(out, in0, s1, s2, op0, op1), R, W)
        else:
            self.op(eng, lambda e: e.tensor_scalar(out, in0, s1, s2, op0, op1, accum_out=accum), R, W)

    def tt(self, eng, out, in0, in1, op, R, W):
        self.op(eng, lambda e: e.tensor_tensor(out, in0, in1, op), R, W)

    def stt(self, eng, out, in0, scalar, in1, op0, op1, R, W):
        self.op(eng, lambda e: e.scalar_tensor_tensor(out, in0, scalar, in1, op0, op1), R, W)

    def cp(self, eng, out, in_, R, W):
        if eng == "act":
            self.op("act", lambda e: e.copy(out, in_), R, W)
        else:
            self.op(eng, lambda e: e.tensor_copy(out, in_), R, W)

    def memset(self, eng, ap, v, W):
        self.op(eng, lambda e: e.memset(ap, v), (), W)


def build(nb, stop=None, dbg=False):
    nc = bass.Bass("TRN2", target_bir_lowering=False)
    dt = nc.dram_tensor

    def din(name, shape, dtype=F32):
        return dt(name, list(shape), dtype, kind="ExternalInput").ap()

    class _L:
        specs = {
            "x": ([nb, S, D], F32), "cT": ([nb, 128, KC], F32), "pos": ([nb, S], I32),
            "ada_w": ([D, 6 * D], F32), "ada_b": ([1, 6 * D], F32), "norm1_g": ([1, D], F32), "norm2_g": ([1, D], F32),
            "w_in": ([D, IN_COLS], F32), "w_sw": ([D, 1152], F32), "b_fgt": ([8, 1], F32), "b_gate": ([128, 16], F32),
            "gains": ([64, 8], F32), "w_proj_fox": ([512, D], F32), "w_proj_dsa": ([512, D], F32), "w_out": ([D, D], F32),
            "w_router": ([D, 36], F32), "b_router": ([1, 36], F32), "exp_w1": ([NE, D, 512], F32),
            "exp_w3": ([NE, D, 512], F32), "exp_w2": ([NE, 512, D], F32), "ident": ([128, 128], F32),
            "trib": ([128, 128], F32), "sel": ([32, NE * 128], F32),
        }
        got = {}

        def __call__(self, name):
            if name not in self.got:
                shp, dty = self.specs[name]
                self.got[name] = din(name, shp, dty)
            return self.got[name]
    L = _L()
    _L.got = {}
    y_d = dt("y", [nb, S, D], F32, kind="ExternalOutput").ap()
    dbg_d = {}

    with ExitStack() as es:
        kb = KB(nc, es)

        uid = [0]

        def sb(name, shape, dtype, st=None):
            uid[0] += 1
            return (st or es).enter_context(nc.sbuf_tensor("sb%d_%s" % (uid[0], name), list(shape), dtype))

        def dbg_out(name, ap, shape, R, dtype=F32):
            if not dbg:
                return
            d = dt("dbg_" + name, list(shape), dtype, kind="ExternalOutput").ap()
            dbg_d[name] = d
            kb.dma("sp", d, ap, R=R)

        PS = [es.enter_context(nc.psum_tensor("ps%d" % i, [128, 512], F32)) for i in range(8)]
        PT = [T() for _ in range(8)]

        ident_f = sb("ident_f", [128, 128], F32)
        ident_b = sb("ident_b", [128, 128], BF16)
        ident4 = sb("ident4", [128, 4, 128], BF16)
        trib = sb("trib", [128, 128], BF16)
        ones_b = sb("ones_b", [128, 64], BF16)
        ones_f = sb("ones_f", [1, 128], F32)
        bd_ones = sb("bd_ones", [128, 128], BF16)
        gains = sb("gains", [128, 8], F32)
        gq8 = sb("gq8", [128, 4], F32)
        bfgt = sb("bfgt", [8, 1], F32)
        nbfgt = sb("nbfgt", [8, 1], F32)
        bgate = sb("bgate", [128, 16], F32)
        wr_f = sb("wr_f", [128, KC, 36], F32)
        brb = sb("brb", [128, 36], F32)
        wr_hi = sb("wr_hi", [128, KC, 36], BF16)
        wr_lo = sb("wr_lo", [128, KC, 36], BF16)
        cT = T()
        with ExitStack() as ph:
            trib_f = sb("trib_f", [128, 128], F32, ph)
            kb.dma("sp", ident_f[:], L("ident"), W=[cT])
            kb.dma("sp", trib_f[:], L("trib"), W=[cT])
            kb.dma("sp", gains[0:64, :], L("gains"), W=[cT])
            kb.dma("sp", gains[64:128, :], L("gains"), W=[cT])
            kb.dma("sp", bfgt[:], L("b_fgt"), W=[cT])
            kb.dma("sp", bgate[:], L("b_gate"), W=[cT])
            kb.dma("sp", brb[:], L("b_router").partition_broadcast(128), W=[cT])
            kb.dma("sp", wr_f[:], L("w_router").rearrange("(k p) n -> p k n", p=128), W=[cT])
            kb.cp("dve", ident_b[:], ident_f[:], [cT], [cT])
            for j in range(4):
                kb.cp("dve", ident4[:, j, :], ident_f[:], [cT], [cT])
            kb.cp("dve", trib[:], trib_f[:], [cT], [cT])
            kb.memset("dve", ones_b[:], 1.0, [cT])
            kb.memset("dve", ones_f[:], 1.0, [cT])
            kb.memset("dve", bd_ones[:], 0.0, [cT])
            kb.memset("dve", bd_ones[0:64, 0:64], 1.0, [cT])
            kb.memset("dve", bd_ones[64:128, 64:128], 1.0, [cT])
            kb.ts("dve", gq8[:, 0:1], gains[:, 0:1], 0.125, None, ALU.mult, None, [cT], [cT])
            kb.ts("dve", gq8[:, 1:2], gains[:, 2:3], 0.125, None, ALU.mult, None, [cT], [cT])
            kb.ts("dve", gq8[:, 2:3], gains[:, 4:5], 0.125, None, ALU.mult, None, [cT], [cT])
            kb.ts("dve", nbfgt[:], bfgt[:], -1.0, None, ALU.mult, None, [cT], [cT])
            kb.cp("dve", wr_hi[:], wr_f[:], [cT], [cT])
            kb.tt("dve", wr_f[:], wr_f[:], wr_hi[:], ALU.subtract, [cT], [cT])
            kb.cp("dve", wr_lo[:], wr_f[:], [cT], [cT])
            kb.barrier()

        modb = sb("modb", [128, 6 * D], F32)
        modT = T()
        win_v = L("w_in").rearrange("(k p) n -> p k n", p=128)
        yT = [[T() for _ in range(NT)] for _ in range(nb)]
        done = False

        for b in range(nb):
            with ExitStack() as ph:
                cact = sb("cact", [128, KC], F32, ph)
                crep = sb("crep", [128, KC, 128], F32, ph)
                n1gb = sb("n1gb", [128, D], F32, ph)
                n2gb = sb("n2gb", [128, D], F32, ph)
                awt = [sb("awt%d" % i, [128, KC, 512], F32, ph) for i in range(2)]
                abt = [sb("abt%d" % i, [1, 512], F32, ph) for i in range(2)]
                awT = [T(), T()]
                cTk = T()
                kb.dma("sp", n1gb[:], L("norm1_g").partition_broadcast(128), W=[cTk])
                kb.dma("sp", n2gb[:], L("norm2_g").partition_broadcast(128), W=[cTk])
                kb.dma("sp", cact[:], L("cT")[b], W=[cTk])
                kb.act(cact[:], cact[:], AF.Silu, [cTk], [cTk])
                for k in range(KC):
                    kb.cp("dve", crep[:, k, :], cact[:, k:k + 1].to_broadcast([128, 128]), [cTk], [cTk])
                adaw_v = L("ada_w").rearrange("(k p) n -> p k n", p=128)
                for g in range(12):
                    i = g % 2
                    kb.dma("sp", awt[i][:], adaw_v[:, :, g * 512:(g + 1) * 512], W=[awT[i]])
                    kb.dma("sp", abt[i][:], L("ada_b")[:, g * 512:(g + 1) * 512], W=[awT[i]])
                    pi = g % 2
                    for k in range(KC):
                        kb.mm(PS[pi][:, :], crep[:, k, :], awt[i][:, k, :], k == 0, False, [cTk, awT[i]], [PT[pi]])
                    kb.mm(PS[pi][:, :], ones_f[0:1, :], abt[i][:], False, True, [awT[i], cT], [PT[pi]])
                    kb.cp("act", modb[:, g * 512:(g + 1) * 512], PS[pi][:, :], [PT[pi]], [modT])
                kb.stt("dve", modb[:, D:2 * D], modb[:, D:2 * D], 1.0, n1gb[:], ALU.add, ALU.mult, [modT, cTk], [modT])
                kb.stt("dve", modb[:, 4 * D:5 * D], modb[:, 4 * D:5 * D], 1.0, n2gb[:], ALU.add, ALU.mult, [modT, cTk], [modT])
                if b == 0:
                    dbg_out("mod", modb[0:1, :], [1, 6 * D], [modT])
                kb.barrier()
            if stop == "M":
                break

            with ExitStack() as pb:
                hT = sb("hT", [128, KC, S], BF16, pb)
                hTT = [T() for _ in range(NCH)]
                WTT = sb("WTT", [32, S], F32, pb)
                WTTT = T()
                with ExitStack() as pa:
                    OFT = sb("OFT", [128, 4, S], BF16, pa)
                    ODT = sb("ODT", [128, 4, S], BF16, pa)
                    OFTT, ODTT = T(), T()
                    with ExitStack() as ph:
                        xt = [sb("xt%d" % i, [128, D], F32, ph) for i in range(2)]
                        xtT = [T(), T()]
                        junk = sb("junk", [128, D], BF16, ph)
                        jT = T()
                        st = sb("stat", [128, 4], F32, ph)
                        sT = T()
                        tmp = sb("tmp", [128, D], F32, ph)
                        tmpT = T()
                        hb = [sb("hb%d" % i, [128, D], BF16, ph) for i in range(2)]
                        hbT = [T(), T()]
                        for t in range(NT):
                            i = t % 2
                            kb.dma("sp", xt[i][:], L("x")[b, t * 128:(t + 1) * 128, :], W=[xtT[i]])
                            kb.act(junk[:], xt[i][:], AF.Square, [xtT[i]], [jT, sT], accum=st[:, 0:1])
                            kb.act(st[:, 1:2], st[:, 0:1], AF.Sqrt, [sT], [sT], bias=EPS, scale=1.0 / D)
                            kb.op("dve", lambda e: e.reciprocal(st[:, 2:3], st[:, 1:2]), [sT], [sT])
                            kb.stt("dve", tmp[:], xt[i][:], st[:, 2:3], modb[:, D:2 * D], ALU.mult, ALU.mult,
                                   [xtT[i], sT, modT], [tmpT])
                            kb.tt("pool", hb[i][:], tmp[:], modb[:, 0:D], ALU.add, [tmpT, modT], [hbT[i]])
                            for j in range(2):
                                pv = PS[6 + j][:, 0:256].bitcast(BF16).rearrange("p (a c) -> p a c", a=4)
                                for a in range(4):
                                    k = 4 * j + a
                                    kb.tr(pv[:, a, :], hb[i][:, k * 128:(k + 1) * 128], ident_b[:], [hbT[i], cT], [PT[6 + j]])
                                kb.cp("act" if j == 0 else "dve", hT[:, 4 * j:4 * j + 4, t * 128:(t + 1) * 128], pv,
                                      [PT[6 + j]], [hTT[t // 4]])
                        if b == 0:
                            dbg_out("hT", hT[:, 0, :], [128, S], hTT, BF16)
                        kb.barrier()
                    if stop == "N1":
                        break

                    with ExitStack() as ph:
                        FQT = sb("FQT", [70, 4, S], BF16, ph)
                        FKT = sb("FKT", [70, 4, S], BF16, ph)
                        FV = sb("FV", [128, NT, 512], BF16, ph)
                        G = sb("G", [8, S], F32, ph)
                        FQTT, FKTT, FVT, GT = T(), T(), T(), T()
                        with ExitStack() as p2:
                            wq = [sb("wq%d" % i, [128, KC, 512], BF16, p2) for i in range(1)]
                            wqT = [T()]
                            Gx = sb("Gx", [8, S], F32, p2)
                            wfl = sb("wfl", [128, KC, 8], BF16, p2)
                            kb.dma("pool", wq[0][:], win_v[:, :, C_FV:C_FV + 512], W=[wqT[0]])
                            for t in range(NT):
                                pi = t % 2
                                for k in range(KC):
                                    kb.mm(PS[pi][:, :], hT[:, k, t * 128:(t + 1) * 128], wq[0][:, k, :], k == 0, k == KC - 1,
                                          [wqT[0], hTT[t // 4]], [PT[pi]])
                                kb.cp("act" if pi == 0 else "dve", FV[:, t, :], PS[pi][:, :], [PT[pi]], [FVT])
                            kb.dma("pool", wfl[:], win_v[:, :, C_FL:C_FL + 8], W=[GT])
                            for c in range(NCH):
                                pi = 4 + c % 2
                                for k in range(KC):
                                    kb.mm(PS[pi][0:8, :], wfl[:, k, :], hT[:, k, c * 512:(c + 1) * 512], k == 0, k == KC - 1,
                                          [GT, hTT[c]], [PT[pi]])
                                kb.act(Gx[:, c * 512:(c + 1) * 512], PS[pi][0:8, :], AF.Exp, [PT[pi], cT], [GT], bias=nbfgt[:], scale=-1.0)
                            kb.act(Gx[:], Gx[:], AF.Ln, [GT], [GT], bias=1.0, scale=1.0)
                            kb.op("dve", lambda e: e.tensor_tensor_scan(G[:], Gx[:], Gx[:], 0.0, ALU.add, ALU.max), [GT], [GT])
                            if b == 0:
                                dbg_out("G", G[:], [8, S], [GT])
                            kb.barrier()
                        pbuf = [sb("pbuf%d" % i, [128, 512], BF16, ph) for i in range(3)]
                        pbT = [T() for _ in range(3)]
                        rinv = sb("rinv", [128, 512], F32, ph)
                        rT = T()
                        for hh in range(2):
                            with ExitStack() as p2:
                                wq = [sb("wq%d" % i, [128, KC, 256], BF16, p2) for i in range(2)]
                                wqT = [T(), T()]
                                sq = sb("sq", [64, 512], BF16, p2)
                                sqT = T()
                                rs = sb("rs", [64, 512], F32, p2)
                                rsT = T()
                                Gy = sb("Gy", [8, S], F32, p2)
                                Gs = [sb("Gs%d" % i, [8, S], BF16, p2) for i in range(2)]
                                GsT = T()
                                for qi, (cbase, dst, dstT, gcol) in enumerate(((C_FQ, FQT, FQTT, gq8[0:64, 0:1]), (C_FK, FKT, FKTT, gains[0:64, 1:2]))):
                                    kb.dma("pool", wq[qi][:], win_v[:, :, cbase + hh * 256:cbase + hh * 256 + 256], W=[wqT[qi]])
                                    for h in range(4):
                                        for c in range(NCH):
                                            pi = (h * NCH + c) % 2
                                            for k in range(KC):
                                                kb.mm(PS[pi][0:64, :], wq[qi][:, k, h * 64:(h + 1) * 64], hT[:, k, c * 512:(c + 1) * 512],
                                                      k == 0, k == KC - 1, [wqT[qi], hTT[c]], [PT[pi]])
                                            kb.act(sq[:], PS[pi][0:64, :], AF.Square, [PT[pi]], [sqT])
                                            kb.mm(PS[2 + pi][0:64, :], ones_b[0:64, :], sq[:], True, True, [sqT, cT], [PT[2 + pi]])
                                            kb.act(rs[:], PS[2 + pi][0:64, :], AF.Sqrt, [PT[2 + pi]], [rsT], bias=EPS, scale=1.0 / 64)
                                            kb.op("dve", lambda e: e.reciprocal(rs[:], rs[:]), [rsT], [rsT])
                                            kb.stt("dve", dst[0:64, h, c * 512:(c + 1) * 512], PS[pi][0:64, :], gcol, rs[:],
                                                   ALU.mult, ALU.mult, [PT[pi], rsT, cT], [dstT])
                                kb.memset("dve", FQT[64:70, :, :], 1.0, [FQTT])
                                kb.memset("dve", FKT[64:70, :, :], 1.0, [FKTT])
                                for j in range(3):
                                    src = G if j == 0 else Gy
                                    kb.cp("dve", Gs[0][:], src[:], [GT, GsT], [GsT])
                                    kb.ts("dve", Gs[1][:], Gs[0][:], -1.0, None, ALU.mult, None, [GsT], [GsT])
                                    for h in range(4):
                                        kb.dma("sp", FQT[64 + j:65 + j, h, :], Gs[1][4 * hh + h:4 * hh + h + 1, :], R=[GsT], W=[FQTT])
                                        kb.dma("sp", FKT[67 + j:68 + j, h, :], Gs[0][4 * hh + h:4 * hh + h + 1, :], R=[GsT], W=[FKTT])
                                    if j < 2:
                                        kb.tt("dve", Gy[:], src[:], Gs[0][:], ALU.subtract, [GT, GsT], [GsT])
                                if b == 0 and hh == 0:
                                    dbg_out("FQT0", FQT[:, 0, :], [70, S], [FQTT], BF16)
                                    dbg_out("FKT0", FKT[:, 0, :], [70, S], [FKTT], BF16)
                                kb.barrier()
                            u = 0
                            for h in range(4):
                                for c in range(NCH):
                                    po = 2 + (h * NCH + c) % 2
                                    pl = 4 + (h * NCH + c) % 2
                                    osl = slice(64 * hh, 64 * hh + 64)
                                    nkt = 4 * c + 4
                                    for kt in range(nkt):
                                        j = kt - 4 * c
                                        c0 = 128 * j if j >= 0 else 0
                                        ps = u % 2
                                        pbi = u % 3
                                        u += 1
                                        kb.mm(PS[ps][:, c0:512], FKT[0:70, h, kt * 128:(kt + 1) * 128],
                                              FQT[0:70, h, c * 512 + c0:(c + 1) * 512], True, j < 0, [FKTT, FQTT], [PT[ps]])
                                        if j >= 0:
                                            kb.mm(PS[ps][:, c0:c0 + 128], trib[:], ident_b[:], False, True, [cT], [PT[ps]])
                                        kb.act(pbuf[pbi][:, c0:512], PS[ps][:, c0:512], AF.Exp, [PT[ps]], [pbT[pbi]])
                                        hg = 4 * hh + h
                                        kb.mm(PS[po][osl, c0:512], FV[:, kt, hg * 64:(hg + 1) * 64], pbuf[pbi][:, c0:512],
                                              kt == 0, kt == nkt - 1, [FVT, pbT[pbi]], [PT[po]])
                                        kb.mm(PS[pl][osl, c0:512], ones_b[:, :], pbuf[pbi][:, c0:512],
                                              kt == 0, kt == nkt - 1, [cT, pbT[pbi]], [PT[pl]])
                                    kb.op("dve", lambda e: e.reciprocal(rinv[osl, :], PS[pl][osl, :]), [PT[pl]], [rT])
                                    kb.tt("dve", OFT[osl, h, c * 512:(c + 1) * 512], PS[po][osl, :], rinv[osl, :], ALU.mult,
                                          [PT[po], rT], [OFTT])
                            kb.barrier()
                        if b == 0:
                            dbg_out("OFT", OFT[:], [128, 4, S], [OFTT], BF16)
                        kb.barrier()
                    if stop == "A1":
                        break

                    with ExitStack() as ph:
                        DQT = sb("DQT", [128, 4, S], BF16, ph)
                        DK = [sb("DK%d" % i, [128, S], BF16, ph) for i in range(2)]
                        DV = sb("DV", [128, NT, 64], BF16, ph)
                        IQT = sb("IQT", [128, 4, S], BF16, ph)
                        IK = [sb("IK%d" % i, [128, S], BF16, ph) for i in range(2)]
                        IW = sb("IW", [128, NT, 8], F32, ph)
                        tabT, DQTT, DKTT, DVT, IQTT, IKTT, IWT = T(), T(), T(), T(), T(), T(), T()
                        with ExitStack() as p1:
                            CS = sb("CS", [128, S], F32, p1)
                            SN = sb("SN", [128, S], F32, p1)
                            with ExitStack() as p2:
                                posi = sb("posi", [128, S], I32, p2)
                                ra = sb("ra", [128, S], F32, p2)
                                ua = sb("ua", [128, S], F32, p2)
                                na = sb("na", [128, S], F32, p2)
                                kb.dma("sp", posi[:], L("pos")[b:b + 1, :].partition_broadcast(128), W=[tabT])
                                kb.cp("dve", ra[:], posi[:], [tabT], [tabT])
                                kb.ts("dve", ra[:], ra[:], gains[:, 6:7], 1.0 / TWO_PI, ALU.mult, ALU.mult, [tabT, cT], [tabT])
                                for which, dst in ((0, SN), (1, CS)):
                                    kb.ts("dve", ua[:], ra[:], 0.25 * which, None, ALU.add, None, [tabT], [tabT])
                                    kb.cp("dve", posi[:], ua[:], [tabT], [tabT])
                                    kb.cp("dve", na[:], posi[:], [tabT], [tabT])
                                    kb.tt("dve", ua[:], ua[:], na[:], ALU.subtract, [tabT], [tabT])
                                    kb.stt("dve", na[:], ua[:], 0.5, ua[:], ALU.is_gt, ALU.subtract, [tabT], [tabT])
                                    kb.stt("dve", ua[:], na[:], 0.5, na[:], ALU.is_gt, ALU.subtract, [tabT], [tabT])
                                    kb.act(dst[:], ua[:], AF.Sin, [tabT], [tabT], scale=TWO_PI * (1.0 - 1e-6))
                                kb.ts("dve", SN[:], SN[:], gains[:, 7:8], None, ALU.mult, None, [tabT, cT], [tabT])
                                if b == 0:
                                    dbg_out("CS", CS[0:64, :], [64, S], [tabT])
                                    dbg_out("SN", SN[0:64, :], [64, S], [tabT])
                                kb.barrier()
                            if stop == "T":
                                break
                            wsw_v = L("w_sw").rearrange("(k p) n -> p k n", p=128)
                            with ExitStack() as p2:
                                wA = sb("wA", [128, KC, 512], BF16, p2)
                                wB = sb("wB", [128, KC, 512], BF16, p2)
                                wA1 = sb("wA1", [128, KC, 64], BF16, p2)
                                wB1 = sb("wB1", [128, KC, 64], BF16, p2)
                                wiw = sb("wiw", [128, KC, 8], BF16, p2)
                                wT = T()
                                sq = sb("sq", [128, 512], BF16, p2)
                                rs = sb("rs", [128, 512], F32, p2)
                                t1 = sb("t1", [128, 512], F32, p2)
                                t2 = sb("t2", [128, 512], F32, p2)
                                t3 = sb("t3", [128, 512], BF16, p2)
                                sqT, rsT, t1T, t2T, t3T = T(), T(), T(), T(), T()
                                for i2 in range(2):
                                    kb.memset("pool", DK[i2][:], 0.0, [DKTT])
                                    kb.memset("pool", IK[i2][:], 0.0, [IKTT])

                                def rope_proj(cA, cB, nheads, dst3, dst2, dstT, gA, gB, norm):
                                    ncol = 64 * nheads
                                    wa, wb = (wA, wB) if nheads == 8 else (wA1, wB1)
                                    kb.dma("pool", wa[:, :, 0:ncol], win_v[:, :, cA:cA + ncol], W=[wT])
                                    kb.dma("pool", wb[:, :, 0:ncol], wsw_v[:, :, cB:cB + ncol], W=[wT])
                                    pairs = [(h, h + 4) for h in range(4)] if nheads == 8 else [(0, 0)]
                                    for (hl, hu) in pairs:
                                        for c in range(NCH):
                                            pi = c % 2
                                            csl = slice(c * 512, (c + 1) * 512)
                                            for (hh_, p0) in ((hl, 0), (hu, 64)):
                                                osl = slice(p0, p0 + 64)
                                                for k in range(KC):
                                                    kb.mm(PS[pi][osl, :], wa[:, k, hh_ * 64:(hh_ + 1) * 64], hT[:, k, csl], k == 0, k == KC - 1,
                                                          [wT, hTT[c]], [PT[pi]])
                                                for k in range(KC):
                                                    kb.mm(PS[2 + pi][osl, :], wb[:, k, hh_ * 64:(hh_ + 1) * 64], hT[:, k, csl], k == 0, k == KC - 1,
                                                          [wT, hTT[c]], [PT[2 + pi]])
                                            if norm:
                                                kb.act(sq[:], PS[pi][:, :], AF.Square, [PT[pi]], [sqT])
                                                kb.mm(PS[4 + pi][:, :], bd_ones[:], sq[:], True, True, [sqT, cT], [PT[4 + pi]])
                                                kb.act(rs[:], PS[4 + pi][:, :], AF.Sqrt, [PT[4 + pi]], [rsT], bias=EPS, scale=1.0 / 64)
                                                kb.op("dve", lambda e: e.reciprocal(rs[:], rs[:]), [rsT], [rsT])
                                                kb.stt("dve", t1[:], PS[pi][:, :], gA, rs[:], ALU.mult, ALU.mult, [PT[pi], rsT, cT], [t1T])
                                                kb.stt("dve", t2[:], PS[2 + pi][:, :], gB, rs[:], ALU.mult, ALU.mult, [PT[2 + pi], rsT, cT], [t2T])
                                                kb.tt("pool", t1[:], t1[:], CS[:, csl], ALU.mult, [t1T, tabT], [t1T])
                                                kb.tt("pool", t2[:], t2[:], SN[:, csl], ALU.mult, [t2T, tabT], [t2T])
                                            else:
                                                kb.tt("dve", t1[:], PS[pi][:, :], CS[:, csl], ALU.mult, [PT[pi], tabT], [t1T])
                                                kb.tt("dve", t2[:], PS[2 + pi][:, :], SN[:, csl], ALU.mult, [PT[2 + pi], tabT], [t2T])
                                            if dst3 is not None:
                                                kb.tt("pool", dst3[:, hl, csl], t1[:], t2[:], ALU.add, [t1T, t2T], [dstT])
                                            else:
                                                kb.tt("pool", t3[:], t1[:], t2[:], ALU.add, [t1T, t2T], [t3T])
                                                kb.cp("dve", dst2[0][0:64, csl], t3[0:64, :], [t3T], [dstT])
                                                kb.cp("dve", dst2[1][64:128, csl], t3[64:128, :], [t3T], [dstT])

                                rope_proj(C_DQ, 0, 8, DQT, None, DQTT, gq8[:, 1:2], gq8[:, 2:3], True)
                                rope_proj(C_DK, 512, 1, None, DK, DKTT, gains[:, 3:4], gains[:, 5:6], True)
                                rope_proj(C_IQ, 576, 8, IQT, None, IQTT, None, None, False)
                                rope_proj(C_IK, 1088, 1, None, IK, IKTT, None, None, False)
                                kb.dma("pool", wA1[:], win_v[:, :, C_DV:C_DV + 64], W=[wT])
                                kb.dma("pool", wiw[:], win_v[:, :, C_IW:C_IW + 8], W=[wT])
                                for t in range(NT):
                                    pi = t % 2
                                    for k in range(KC):
                                        kb.mm(PS[pi][:, 0:64], hT[:, k, t * 128:(t + 1) * 128], wA1[:, k, :], k == 0, k == KC - 1,
                                              [wT, hTT[t // 4]], [PT[pi]])
                                    for k in range(KC):
                                        kb.mm(PS[2 + pi][:, 0:8], hT[:, k, t * 128:(t + 1) * 128], wiw[:, k, :], k == 0, k == KC - 1,
                                              [wT, hTT[t // 4]], [PT[2 + pi]])
                                    kb.cp("act", DV[:, t, :], PS[pi][:, 0:64], [PT[pi]], [DVT])
                                    kb.cp("dve", IW[:, t, :], PS[2 + pi][:, 0:8], [PT[2 + pi]], [IWT])
                                if b == 0:
                                    dbg_out("DQT", DQT[:], [128, 4, S], [DQTT], BF16)
                                    dbg_out("DKT", DK[0][:], [128, S], [DKTT], BF16)
                                    dbg_out("IQT", IQT[:], [128, 4, S], [IQTT], BF16)
                                    dbg_out("IKT", IK[1][:], [128, S], [IKTT], BF16)
                                    dbg_out("IW", IW[:], [128, NT, 8], [IWT])
                                kb.barrier()
                        if stop in ("P2", "T"):
                            break
                        scb = [sb("scb%d" % i, [128, S], F32, ph) for i in range(2)]
                        scT = [T(), T()]
                        MB = [sb("MB%d" % i, [128, S], BF16, ph) for i in range(2)]
                        MBT = [T(), T()]
                        junk = sb("junkb", [128, S], BF16, ph)
                        jT = T()
                        rbuf = [sb("rbuf%d" % i, [128, 512], F32, ph) for i in range(2)]
                        rbT = [T(), T()]
                        bs = sb("bs", [128, 8], F32, ph)
                        bsT = T()
                        pbuf = [sb("pbufd%d" % i, [128, 512], BF16, ph) for i in range(3)]
                        pbT = [T() for _ in range(3)]
                        rinv = sb("rinvd", [128, 512], F32, ph)
                        rT = T()
                        u = 0
                        ur = 0
                        for i in range(NT):
                            end = 128 * (i + 1)
                            sc = scb[i % 2]
                            sT_ = scT[i % 2]
                            qsl = slice(i * 128, (i + 1) * 128)
                            ng = (end + 511) // 512
                            for h in range(8):
                                hs = slice(64 * (h // 4), 64 * (h // 4) + 64)
                                for g in range(ng):
                                    n = min(512, end - 512 * g)
                                    ps = ur % 2
                                    ri = ur % 2
                                    ur += 1
                                    kb.mm(PS[ps][:, 0:n], IQT[:, h % 4, qsl], IK[h // 4][:, g * 512:g * 512 + n], True, True,
                                          [IQTT, IKTT], [PT[ps]])
                                    kb.act(rbuf[ri][:, 0:n], PS[ps][:, 0:n], AF.Relu, [PT[ps]], [rbT[ri]])
                                    if h == 0:
                                        kb.ts("dve", sc[:, g * 512:g * 512 + n], rbuf[ri][:, 0:n], IW[:, i, 0:1], None, ALU.mult, None,
                                              [rbT[ri], IWT], [sT_])
                                    else:
                                        kb.stt("dve", sc[:, g * 512:g * 512 + n], rbuf[ri][:, 0:n], IW[:, i, h:h + 1],
                                               sc[:, g * 512:g * 512 + n], ALU.mult, ALU.add, [rbT[ri], IWT, sT_], [sT_])
                            if i >= 2:
                                kb.op("dve", lambda e: e.tensor_reduce(bs[:, 0:1], sc[:, 0:end], AX.X, ALU.max), [sT_], [bsT])
                                kb.op("dve", lambda e: e.tensor_reduce(bs[:, 1:2], sc[:, 0:end], AX.X, ALU.min), [sT_], [bsT])
                                kb.tt("dve", bs[:, 2:3], bs[:, 0:1], bs[:, 1:2], ALU.subtract, [bsT], [bsT])
                                kb.memset("dve", sc[0:64, end - 64:end], -1e30, [sT_])
                                for it in range(N_BISECT):
                                    f = 2.0 ** (-(it + 1))
                                    kb.stt("dve", bs[:, 3:4], bs[:, 2:3], f, bs[:, 1:2], ALU.mult, ALU.add, [bsT], [bsT])
                                    kb.ts("dve", junk[:, 0:end], sc[:, 0:end], bs[:, 3:4], None, ALU.is_ge, ALU.add, [sT_, bsT], [jT, bsT],
                                          accum=bs[:, 4:5])
                                    kb.ts("dve", bs[:, 5:6], bs[:, 4:5], 256.0, f, ALU.is_ge, ALU.mult, [bsT], [bsT])
                                    kb.stt("dve", bs[:, 1:2], bs[:, 5:6], bs[:, 2:3], bs[:, 1:2], ALU.mult, ALU.add, [bsT], [bsT])
                            else:
                                kb.memset("dve", bs[:, 1:2], -1e29, [bsT])
                                kb.memset("dve", sc[0:64, end - 64:end], -1e30, [sT_])
                            mb = MB[i % 2]
                            mT_ = MBT[i % 2]
                            kb.ts("dve", mb[:, 0:end], sc[:, 0:end], bs[:, 1:2], NEG, ALU.is_lt, ALU.mult, [sT_, bsT], [mT_])
                            if b == 0 and i in (1, 5):
                                dbg_out("MB%d" % i, mb[:, 0:end], [128, end], [mT_], BF16)
                                dbg_out("SC%d" % i, sc[:, 0:end], [128, end], [sT_])
                            for hg in range(2):
                                hs = slice(64 * hg, 64 * hg + 64)
                                po = 2 + (2 * i + hg) % 2
                                pl = 4 + (2 * i + hg) % 2
                                for kt in range(i + 1):
                                    ps = 6 + u % 2
                                    pbi = u % 3
                                    u += 1
                                    ksl = slice(kt * 128, (kt + 1) * 128)
                                    psv = PS[ps][:, :].rearrange("p (a c) -> p a c", a=4)
                                    kb.mm(psv, DK[hg][:, ksl], DQT[:, :, qsl], True, False, [DKTT, DQTT], [PT[ps]])
                                    kb.mm(psv, mb[:, ksl], ident4[:], False, True, [mT_, cT], [PT[ps]])
                                    kb.act(pbuf[pbi][:], PS[ps][:, :], AF.Exp, [PT[ps]], [pbT[pbi]])
                                    kb.mm(PS[po][hs, :], DV[:, kt, :], pbuf[pbi][:], kt == 0, kt == i, [DVT, pbT[pbi]], [PT[po]])
                                    kb.mm(PS[pl][hs, :], ones_b[:, :], pbuf[pbi][:], kt == 0, kt == i, [cT, pbT[pbi]], [PT[pl]])
                                kb.op("dve", lambda e: e.reciprocal(rinv[hs, :], PS[pl][hs, :]), [PT[pl]], [rT])
                                kb.tt("dve", ODT[hs, :, qsl], PS[po][hs, :].rearrange("p (a c) -> p a c", a=4),
                                      rinv[hs, :].rearrange("p (a c) -> p a c", a=4), ALU.mult, [PT[po], rT], [ODTT])
                        if b == 0:
                            dbg_out("ODT", ODT[:], [128, 4, S], [ODTT], BF16)
                        kb.barrier()
                    if stop == "A2":
                        break

                    with ExitStack() as ph:
                        MT = sb("MT", [128, KC, S], BF16, ph)
                        MTT = [T() for _ in range(NCH)]
                        with ExitStack() as p2:
                            wpf = sb("wpf", [128, 4, D], BF16, p2)
                            wpd = sb("wpd", [128, 4, D], BF16, p2)
                            wstg = sb("wstg", [128, 4, D], F32, p2)
                            wpT = T()
                            wsT = T()
                            for nm, dstw in (("w_proj_fox", wpf), ("w_proj_dsa", wpd)):
                                wv = L(nm).rearrange("(a q d) n -> a d q n", a=2, q=4)
                                for a in range(2):
                                    kb.dma("sp", wstg[64 * a:64 * a + 64, :, :], wv[a], W=[wsT])
                                kb.cp("pool", dstw[:], wstg[:], [wsT], [wpT])
                            gwf = [sb("gwf%d" % i, [128, KC, 128], BF16, p2) for i in range(2)]
                            gwd = [sb("gwd%d" % i, [128, KC, 128], BF16, p2) for i in range(2)]
                            gwT = [T(), T()]
                            sf = [sb("sf%d" % i, [128, 512], F32, p2) for i in range(2)]
                            sd = [sb("sd%d" % i, [128, 512], F32, p2) for i in range(2)]
                            sfT = [T(), T()]
                            sdT = [T(), T()]
                            for cc in range(KC):
                                i = cc % 2
                                kb.dma("pool", gwf[i][:], win_v[:, :, C_GF + cc * 128:C_GF + (cc + 1) * 128], W=[gwT[i]])
                                kb.dma("pool", gwd[i][:], win_v[:, :, C_GD + cc * 128:C_GD + (cc + 1) * 128], W=[gwT[i]])
                                for c in range(NCH):
                                    csl = slice(c * 512, (c + 1) * 512)
                                    par = (cc * NCH + c) % 2
                                    b0 = 4 * par
                                    for q in range(4):
                                        kb.mm(PS[b0][:, :], wpf[:, q, cc * 128:(cc + 1) * 128], OFT[:, q, csl], q == 0, q == 3,
                                              [wpT, OFTT], [PT[b0]])
                                    for q in range(4):
                                        kb.mm(PS[b0 + 1][:, :], wpd[:, q, cc * 128:(cc + 1) * 128], ODT[:, q, csl], q == 0, q == 3,
                                              [wpT, ODTT], [PT[b0 + 1]])
                                    for k in range(KC):
                                        kb.mm(PS[b0 + 2][:, :], gwf[i][:, k, :], hT[:, k, csl], k == 0, k == KC - 1,
                                              [gwT[i], hTT[c]], [PT[b0 + 2]])
                                    for k in range(KC):
                                        kb.mm(PS[b0 + 3][:, :], gwd[i][:, k, :], hT[:, k, csl], k == 0, k == KC - 1,
                                              [gwT[i], hTT[c]], [PT[b0 + 3]])
                                    kb.act(sf[par][:], PS[b0 + 2][:, :], AF.Sigmoid, [PT[b0 + 2], cT], [sfT[par]], bias=bgate[:, cc:cc + 1])
                                    kb.act(sd[par][:], PS[b0 + 3][:, :], AF.Sigmoid, [PT[b0 + 3], cT], [sdT[par]], bias=bgate[:, 8 + cc:9 + cc])
                                    kb.tt("dve", sf[par][:], sf[par][:], PS[b0][:, :], ALU.mult, [sfT[par], PT[b0]], [sfT[par]])
                                    kb.tt("dve", sd[par][:], sd[par][:], PS[b0 + 1][:, :], ALU.mult, [sdT[par], PT[b0 + 1]], [sdT[par]])
                                    kb.tt("pool", MT[:, cc, csl], sf[par][:], sd[par][:], ALU.add, [sfT[par], sdT[par]], [MTT[c]])
                            if b == 0:
                                dbg_out("MT", MT[:, 0, :], [128, S], MTT, BF16)
                            kb.barrier()
                        if stop == "G":
                            break
                        with ExitStack() as p2:
                            wo_b = sb("wo_b", [128, KC, D], BF16, p2)
                            woT = T()
                            kb.dma("pool", wo_b[:], L("w_out").rearrange("(k p) n -> p k n", p=128), W=[woT])
                            xt = [sb("xo%d" % i, [128, D], F32, p2) for i in range(2)]
                            xtT = [T(), T()]
                            tmp = sb("tmpo", [128, D], F32, p2)
                            tmpT = T()
                            junk = sb("junko", [128, D], BF16, p2)
                            jT = T()
                            h2f = sb("h2f", [128, D], F32, p2)
                            h2fT = T()
                            h2hi = sb("h2hi", [128, D], BF16, p2)
                            h2lo = sb("h2lo", [128, D], BF16, p2)
                            loTt = sb("loTt", [128, KC, 128], BF16, p2)
                            hiT, loT, loTT = T(), T(), T()
                            st = sb("stato", [128, 4], F32, p2)
                            sT = T()
                            rt = sb("rt", [128, 160], F32, p2)
                            rtT = T()
                            for t in range(NT):
                                i = t % 2
                                tsl = slice(t * 128, (t + 1) * 128)
                                for half in range(2):
                                    for cc in range(KC):
                                        kb.mm(PS[half][:, :], MT[:, cc, tsl], wo_b[:, cc, half * 512:(half + 1) * 512], cc == 0, cc == KC - 1,
                                              [MTT[t // 4], woT], [PT[half]])
                                kb.dma("sp", xt[i][:], L("x")[b, tsl, :], W=[xtT[i]])
                                for half in range(2):
                                    hsl = slice(half * 512, (half + 1) * 512)
                                    kb.tt("dve", tmp[:, hsl], PS[half][:, :], modb[:, 2 * D + half * 512:2 * D + (half + 1) * 512], ALU.mult,
                                          [PT[half], modT], [tmpT])
                                kb.tt("pool", xt[i][:], xt[i][:], tmp[:], ALU.add, [xtT[i], tmpT], [xtT[i]])
                                kb.dma("sp", y_d[b, tsl, :], xt[i][:], R=[xtT[i]], W=[yT[b][t]])
                                if stop == "O1":
                                    continue
                                kb.act(junk[:], xt[i][:], AF.Square, [xtT[i]], [jT, sT], accum=st[:, 0:1])
                                kb.act(st[:, 1:2], st[:, 0:1], AF.Sqrt, [sT], [sT], bias=EPS, scale=1.0 / D)
                                kb.op("dve", lambda e: e.reciprocal(st[:, 2:3], st[:, 1:2]), [sT], [sT])
                                kb.stt("dve", tmp[:], xt[i][:], st[:, 2:3], modb[:, 4 * D:5 * D], ALU.mult, ALU.mult,
                                       [xtT[i], sT, modT], [tmpT])
                                kb.tt("pool", h2f[:], tmp[:], modb[:, 3 * D:4 * D], ALU.add, [tmpT, modT], [h2fT])
                                kb.cp("dve", h2hi[:], h2f[:], [h2fT], [hiT])
                                kb.tt("pool", h2lo[:], h2f[:], h2hi[:], ALU.subtract, [h2fT, hiT], [loT])
                                for j in range(2):
                                    pv = PS[2 + j][:, 0:256].bitcast(BF16).rearrange("p (a c) -> p a c", a=4)
                                    for a in range(4):
                                        k = 4 * j + a
                                        kb.tr(pv[:, a, :], h2hi[:, k * 128:(k + 1) * 128], ident_b[:], [hiT, cT], [PT[2 + j]])
                                    kb.cp("act" if j == 0 else "dve", hT[:, 4 * j:4 * j + 4, tsl], pv, [PT[2 + j]], [hTT[t // 4]])
                                for j in range(2):
                                    pv = PS[6 + j][:, 0:256].bitcast(BF16).rearrange("p (a c) -> p a c", a=4)
                                    for a in range(4):
                                        k = 4 * j + a
                                        kb.tr(pv[:, a, :], h2lo[:, k * 128:(k + 1) * 128], ident_b[:], [loT, cT], [PT[6 + j]])
                                    kb.cp("act" if j == 0 else "dve", loTt[:, 4 * j:4 * j + 4, :], pv, [PT[6 + j]], [loTT])
                                if stop == "O2":
                                    continue
                                nmm = 3 * KC
                                q = 0
                                for (lh, lhT_, wgt) in ((hT, hTT[t // 4], wr_hi), (hT, hTT[t // 4], wr_lo), (loTt, loTT, wr_hi)):
                                    for k in range(KC):
                                        lhs_ap = lh[:, k, tsl] if lh is hT else lh[:, k, :]
                                        kb.mm(PS[4][:, 0:36], lhs_ap, wgt[:, k, :], q == 0, q == nmm - 1, [lhT_, cT], [PT[4]])
                                        q += 1
                                if stop == "O3":
                                    continue
                                R_ = [rtT]
                                lg = rt[:, 0:36]
                                kb.tt("dve", lg, PS[4][:, 0:36], brb[:], ALU.add, [PT[4], cT], R_)
                                kb.op("dve", lambda e: e.tensor_reduce(rt[:, 40:41], rt[:, 0:4], AX.X, ALU.max), R_, R_)
                                kb.ts("dve", rt[:, 44:48], rt[:, 0:4], rt[:, 40:41], None, ALU.is_ge, None, R_, R_)
                                kb.ts("dve", rt[:, 41:42], rt[:, 40:41], -1.0, None, ALU.mult, None, R_, R_)
                                kb.act(rt[:, 48:52], rt[:, 0:4], AF.Exp, R_, R_, bias=rt[:, 41:42], accum=rt[:, 42:43])
                                kb.op("dve", lambda e: e.reciprocal(rt[:, 43:44], rt[:, 42:43]), R_, R_)
                                kb.ts("dve", rt[:, 52:56], rt[:, 44:48], -1.0, 1e30, ALU.add, ALU.mult, R_, R_)
                                for gi in range(4):
                                    kb.ts("dve", rt[:, 60 + 8 * gi:68 + 8 * gi], rt[:, 4 + 8 * gi:12 + 8 * gi], rt[:, 44 + gi:45 + gi],
                                          rt[:, 52 + gi:53 + gi], ALU.mult, ALU.add, R_, R_)
                                lem = rt[:, 60:92]
                                kb.op("dve", lambda e: e.tensor_reduce(rt[:, 56:57], rt[:, 60:92], AX.X, ALU.max), R_, R_)
                                kb.ts("dve", rt[:, 92:124], lem, rt[:, 56:57], None, ALU.is_ge, None, R_, R_)
                                kb.stt("dve", lem, rt[:, 92:124], -1e30, lem, ALU.mult, ALU.add, R_, R_)
                                kb.op("dve", lambda e: e.tensor_reduce(rt[:, 57:58], rt[:, 60:92], AX.X, ALU.max), R_, R_)
                                kb.ts("dve", rt[:, 124:156], lem, rt[:, 57:58], None, ALU.is_ge, None, R_, R_)
                                kb.tt("dve", rt[:, 58:59], rt[:, 57:58], rt[:, 56:57], ALU.subtract, R_, R_)
                                kb.act(rt[:, 59:60], rt[:, 58:59], AF.Exp, R_, R_)
                                kb.ts("dve", rt[:, 36:37], rt[:, 59:60], 1.0, None, ALU.add, None, R_, R_)
                                kb.op("dve", lambda e: e.reciprocal(rt[:, 36:37], rt[:, 36:37]), R_, R_)
                                kb.tt("dve", rt[:, 37:38], rt[:, 36:37], rt[:, 43:44], ALU.mult, R_, R_)
                                kb.tt("dve", rt[:, 38:39], rt[:, 37:38], rt[:, 59:60], ALU.mult, R_, R_)
                                kb.ts("dve", rt[:, 92:124], rt[:, 92:124], rt[:, 37:38], None, ALU.mult, None, R_, R_)
                                kb.stt("dve", rt[:, 92:124], rt[:, 124:156], rt[:, 38:39], rt[:, 92:124], ALU.mult, ALU.add, R_, R_)
                                if stop == "O4":
                                    continue
                                kb.mm(PS[5][0:32, 0:128], rt[:, 92:124], ident_f[:], True, True, [rtT, cT], [PT[5]])
                                kb.cp("act", WTT[:, tsl], PS[5][0:32, 0:128], [PT[5]], [WTTT])
                            if b == 0:
                                dbg_out("WTT", WTT[:], [32, S], [WTTT])
                                dbg_out("h2T", hT[:, 0, :], [128, S], hTT, BF16)
                            kb.barrier()
                if stop in ("O", "O1", "O2", "O3", "O4"):
                    break
                with ExitStack() as ph:
                    XM = sb("XM", [128, NT, D], F32, ph)
                    XMT = [T() for _ in range(NT)]
                    W1 = [sb("W1_%d" % i, [128, KC, 512], BF16, ph) for i in range(2)]
                    W3 = [sb("W3_%d" % i, [128, KC, 512], BF16, ph) for i in range(2)]
                    W2 = [sb("W2_%d" % i, [128, 4, D], BF16, ph) for i in range(2)]
                    wT = [T(), T()]
                    gT = [sb("gT%d" % i, [128, 4, 512], BF16, ph) for i in range(2)]
                    gTT = [T(), T()]
                    sa = [sb("sa%d" % i, [128, 512], F32, ph) for i in range(2)]
                    saT = [T(), T()]
                    g1 = [sb("g1%d" % i, [128, 512], BF16, ph) for i in range(2)]
                    g1T = [T(), T()]
                    wtb = [sb("wtb%d" % i, [128, 512], F32, ph) for i in range(2)]
                    wtbT = [T(), T()]
                    selE = [sb("selE%d" % i, [32, 128], F32, ph) for i in range(2)]
                    selT = [T(), T()]
                    xo = sb("xfin", [128, D], F32, ph)
                    xoT = T()
                    ne_run = NE if stop != "E1" else 2
                    for e in range(ne_run):
                        i = e % 2
                        kb.dma("pool", W1[i][:], L("exp_w1")[e].rearrange("(k p) f -> p k f", p=128), W=[wT[i]])
                        kb.dma("pool", W3[i][:], L("exp_w3")[e].rearrange("(k p) f -> p k f", p=128), W=[wT[i]])
                        kb.dma("pool", W2[i][:], L("exp_w2")[e].rearrange("(k p) n -> p k n", p=128), W=[wT[i]])
                        for fc in range(4):
                            kb.tt("pool", W2[i][:, fc, :], W2[i][:, fc, :], modb[:, 5 * D:6 * D], ALU.mult, [wT[i], modT], [wT[i]])
                        kb.cp("dve", selE[i][:], ident_f[0:32, e:e + 1].to_broadcast([32, 128]), [cT], [selT[i]])
                        for c in range(NCH):
                            csl = slice(c * 512, (c + 1) * 512)
                            par = (e * NCH + c) % 2
                            kb.mm(PS[6][:, :], selE[i][:], WTT[:, csl], True, True, [selT[i], WTTT], [PT[6]])
                            kb.cp("act", wtb[par][:], PS[6][:, :], [PT[6]], [wtbT[par]])
                            for fc in range(4):
                                q = fc % 2
                                fsl = slice(fc * 128, (fc + 1) * 128)
                                for k in range(KC):
                                    kb.mm(PS[2 * q][:, :], W1[i][:, k, fsl], hT[:, k, csl], k == 0, k == KC - 1, [wT[i], hTT[c]], [PT[2 * q]])
                                for k in range(KC):
                                    kb.mm(PS[2 * q + 1][:, :], W3[i][:, k, fsl], hT[:, k, csl], k == 0, k == KC - 1, [wT[i], hTT[c]], [PT[2 * q + 1]])
                                kb.act(sa[q][:], PS[2 * q][:, :], AF.Silu, [PT[2 * q]], [saT[q]])
                                kb.tt("dve", g1[q][:], sa[q][:], PS[2 * q + 1][:, :], ALU.mult, [saT[q], PT[2 * q + 1]], [g1T[q]])
                                kb.tt("pool", gT[par][:, fc, :], g1[q][:], wtb[par][:], ALU.mult, [g1T[q], wtbT[par]], [gTT[par]])
                            for tq in range(4):
                                t = 4 * c + tq
                                for half in range(2):
                                    py = 4 + (2 * tq + half) % 2
                                    hsl = slice(half * 512, (half + 1) * 512)
                                    for fc in range(4):
                                        kb.mm(PS[py][:, :], gT[par][:, fc, tq * 128:(tq + 1) * 128], W2[i][:, fc, hsl], fc == 0, fc == 3,
                                              [gTT[par], wT[i]], [PT[py]])
                                    if e == 0:
                                        kb.cp("dve", XM[:, t, hsl], PS[py][:, :], [PT[py]], [XMT[t]])
                                    else:
                                        kb.tt("dve", XM[:, t, hsl], XM[:, t, hsl], PS[py][:, :], ALU.add, [XMT[t], PT[py]], [XMT[t]])
                    for t in range(NT):
                        tsl = slice(t * 128, (t + 1) * 128)
                        kb.dma("sp", xo[:], y_d[b, tsl, :], R=[yT[b][t]], W=[xoT])
                        kb.tt("pool", xo[:], xo[:], XM[:, t, :], ALU.add, [xoT, XMT[t]], [xoT])
                        kb.dma("sp", y_d[b, tsl, :], xo[:], R=[xoT], W=[yT[b][t]])
                    kb.barrier()
        kb.barrier(("sp",))
    return nc, (dbg_d, set(_L.got))


def _consts():
    ident = np.eye(128, dtype=np.float32)
    q = np.arange(128)[:, None]
    k = np.arange(128)[None, :]
    trib = np.where(k > q, NEG, 0.0).astype(np.float32)
    sel = np.zeros((32, NE, 128), np.float32)
    for e in range(NE):
        sel[e, e, :] = 1.0
    half = 32
    inv = (np.float32(10000.0) ** (-np.arange(half, dtype=np.float32) / np.float32(half))).astype(np.float32)
    invf = np.concatenate([inv, inv]).astype(np.float32)
    sgn = np.concatenate([-np.ones(32, np.float32), np.ones(32, np.float32)])
    return ident, trib, sel.reshape(32, NE * 128), invf, sgn


def _swap_cols(w, base, nheads):
    cols = []
    for h in range(nheads):
        cols.append(w[:, base + h * 64 + 32: base + h * 64 + 64])
        cols.append(w[:, base + h * 64: base + h * 64 + 32])
    return np.concatenate(cols, axis=1)


def prep_shared(inp):
    f = lambda a: np.ascontiguousarray(np.asarray(a, dtype=np.float32))
    ident, trib, sel, invf, sgn = _consts()
    w_in = f(inp["w_in"][0])
    w_sw = np.concatenate([_swap_cols(w_in, C_DQ, 8), _swap_cols(w_in, C_DK, 1),
                           _swap_cols(w_in, C_IQ, 8), _swap_cols(w_in, C_IK, 1)], axis=1)
    sw = lambda g: np.concatenate([g[32:], g[:32]])
    qd, kd = f(inp["qn_dsa"][0]), f(inp["kn_dsa"][0])
    gains = np.stack([f(inp["qn_fox"][0]), f(inp["kn_fox"][0]), qd, kd, sw(qd), sw(kd), invf, sgn], axis=1)
    sh = {
        "ada_w": f(inp["ada_w"][0]), "ada_b": f(inp["ada_b"][0]).reshape(1, -1),
        "norm1_g": f(inp["norm1_g"][0]).reshape(1, -1), "norm2_g": f(inp["norm2_g"][0]).reshape(1, -1),
        "w_in": w_in, "w_sw": f(w_sw), "b_fgt": f(inp["b_fgt"][0]).reshape(8, 1),
        "b_gate": f(f(inp["b_gate"][0]).reshape(16, 128).T), "gains": f(gains),
        "w_proj_fox": f(inp["w_proj_fox"][0]), "w_proj_dsa": f(inp["w_proj_dsa"][0]), "w_out": f(inp["w_out"][0]),
        "w_router": f(np.concatenate([inp["router_w_grp"][0], inp["router_w_exp"][0]], axis=1)),
        "b_router": f(np.concatenate([inp["router_b_grp"][0], inp["router_b_exp"][0]])).reshape(1, 36),
        "exp_w1": f(inp["exp_w1"][0]), "exp_w3": f(inp["exp_w3"][0]), "exp_w2": f(inp["exp_w2"][0]),
        "ident": ident, "trib": trib, "sel": sel,
    }
    return sh


def prep_core(inp, b0, nb):
    x = np.ascontiguousarray(np.asarray(inp["x"][b0:b0 + nb], dtype=np.float32))
    c = np.asarray(inp["c"][b0:b0 + nb], dtype=np.float32)
    cT = np.ascontiguousarray(c.reshape(nb, KC, 128).transpose(0, 2, 1))
    pos = np.ascontiguousarray(np.asarray(inp["positions"][b0:b0 + nb], dtype=np.int32))
    return {"x": x, "cT": cT, "pos": pos}


def kernel(**inputs):
    n = 8
    nb = 4
    nc, (_, used) = build(nb)
    sh = prep_shared(inputs)
    in_maps = []
    for i in range(n):
        m = dict(sh)
        m.update(prep_core(inputs, i * nb, nb))
        in_maps.append({k: v for k, v in m.items() if k in used})
    res = run_bass_kernel_spmd(nc, in_maps, core_ids=list(range(n)))
    return np.concatenate([r["y"] for r in res.results], axis=0).astype(np.float32)
```

```python
import numpy as np
from contextlib import ExitStack
import concourse.bass as bass
import concourse.mybir as mybir
from concourse.bass_utils import run_bass_kernel_spmd

F32 = mybir.dt.float32
BF16 = mybir.dt.bfloat16
I32 = mybir.dt.int32
ALU = mybir.AluOpType
AF = mybir.ActivationFunctionType
AX = mybir.AxisListType

S = 2048
D = 1024
NT = 16
NCH = 4
KC = 8
NE = 32
C_FQ, C_FK, C_FV, C_FL, C_DQ, C_DK, C_DV, C_IQ, C_IK, C_IW, C_GF, C_GD = (
    0, 512, 1024, 1536, 1544, 2056, 2120, 2184, 2696, 2760, 2768, 3792)
IN_COLS = 4816
EPS = 1e-6
NEG = -30000.0
N_BISECT = 22
TWO_PI = 6.283185307179586


class T:
    __slots__ = ("w", "r")

    def __init__(self):
        self.w = {}
        self.r = {}


class KB:
    def __init__(self, nc, es, n_sp=12, n_pool=6):
        self.nc = nc
        self.E = {"pe": nc.tensor, "act": nc.scalar, "dve": nc.vector, "pool": nc.gpsimd, "sp": nc.sync}
        self.semobj = {}
        self.cnt = {}
        for e in ("pe", "act", "dve", "pool"):
            self.semobj[e] = es.enter_context(nc.semaphore("s_" + e))
            self.cnt[e] = 0
        self.known = {e: {} for e in self.E}
        self.dsems = {"sp": [], "pool": []}
        self.dtot = {}
        self.dnext = {"sp": 0, "pool": 0}
        for q, n in (("sp", n_sp), ("pool", n_pool)):
            for i in range(n):
                name = "d_%s%d" % (q, i)
                self.semobj[name] = es.enter_context(nc.semaphore(name))
                self.dsems[q].append(name)
                self.dtot[name] = 0

    @staticmethod
    def _deps(R, W):
        deps = {}
        for t in R:
            for k, v in t.w.items():
                if deps.get(k, 0) < v:
                    deps[k] = v
        for t in W:
            for d in (t.w, t.r):
                for k, v in d.items():
                    if deps.get(k, 0) < v:
                        deps[k] = v
        return deps

    def _wait(self, eng, deps):
        kn = self.known[eng]
        for k, v in deps.items():
            if eng == "pe" and k == "pe":
                continue
            if kn.get(k, 0) >= v:
                continue
            self.E[eng].wait_ge(self.semobj[k], v)
            kn[k] = v

    def op(self, eng, fn, R=(), W=()):
        self._wait(eng, self._deps(R, W))
        ins = fn(self.E[eng])
        self.cnt[eng] += 1
        c = self.cnt[eng]
        ins.then_inc(self.semobj[eng], 1)
        for t in W:
            t.w = {eng: c}
            t.r = {}
        for t in R:
            if t.r.get(eng, 0) < c:
                t.r[eng] = c

    def dma(self, q, out, in_, R=(), W=()):
        deps = self._deps(R, W)
        i = self.dnext[q]
        self.dnext[q] = (i + 1) % len(self.dsems[q])
        name = self.dsems[q][i]
        tot = self.dtot[name]
        if deps.get(name, 0) < tot:
            deps[name] = tot
        self._wait(q, deps)
        self.E[q].dma_start(out=out, in_=in_).then_inc(self.semobj[name], 16)
        tot += 16
        self.dtot[name] = tot
        for t in W:
            t.w = {name: tot}
            t.r = {}
        for t in R:
            t.r[name] = tot

    def barrier(self, engs=("pe", "act", "dve", "pool", "sp")):
        allv = dict(self.cnt)
        allv.update(self.dtot)
        allv = {k: v for k, v in allv.items() if v > 0}
        for e in engs:
            self._wait(e, dict(allv))

    def mm(self, out, lhsT, rhs, start, stop, R, W):
        self.op("pe", lambda e: e.matmul(out, lhsT, rhs, start=start, stop=stop), R, W)

    def tr(self, out, in_, ident, R, W):
        self.op("pe", lambda e: e.transpose(out, in_, ident), R, W)

    def act(self, out, in_, func, R, W, bias=0.0, scale=1.0, accum=None):
        if accum is None:
            self.op("act", lambda e: e.activation(out, in_, func, bias=bias, scale=scale), R, W)
        else:
            self.op("act", lambda e: e.activation(out, in_, func, bias=bias, scale=scale, accum_out=accum), R, W)

    def ts(self, eng, out, in0, s1, s2, op0, op1, R, W, accum=None):
        if accum is None:
            if op1 is None:
                self.op(eng, lambda e: e.tensor_scalar(out, in0, s1, None, op0), R, W)
            else:
                self.op(eng, lambda e: e.tensor_scalar(out, in0, s1, s2, op0, op1), R, W)
        else:
            self.op(eng, lambda e: e.tensor_scalar(out, in0, s1, s2, op0, op1, accum_out=accum), R, W)

    def tt(self, eng, out, in0, in1, op, R, W):
        self.op(eng, lambda e: e.tensor_tensor(out, in0, in1, op), R, W)

    def stt(self, eng, out, in0, scalar, in1, op0, op1, R, W):
        self.op(eng, lambda e: e.scalar_tensor_tensor(out, in0, scalar, in1, op0, op1), R, W)

    def cp(self, eng, out, in_, R, W):
        if eng == "act":
            self.op("act", lambda e: e.copy(out, in_), R, W)
        else:
            self.op(eng, lambda e: e.tensor_copy(out, in_), R, W)

    def memset(self, eng, ap, v, W):
        self.op(eng, lambda e: e.memset(ap, v), (), W)


def build(nb, stop=None, dbg=False):
    nc = bass.Bass("TRN2", target_bir_lowering=False)
    dt = nc.dram_tensor

    def din(name, shape, dtype=F32):
        return dt(name, list(shape), dtype, kind="ExternalInput").ap()

    class _L:
        specs = {
            "x": ([nb, S, D], F32), "cT": ([nb, 128, KC], F32), "pos": ([nb, S], I32),
            "ada_w": ([D, 6 * D], F32), "ada_b": ([1, 6 * D], F32), "norm1_g": ([1, D], F32), "norm2_g": ([1, D], F32),
            "w_in": ([D, IN_COLS], F32), "w_sw": ([D, 1152], F32), "b_fgt": ([8, 1], F32), "b_gate": ([128, 16], F32),
            "gains": ([64, 8], F32), "w_proj_fox": ([512, D], F32), "w_proj_dsa": ([512, D], F32), "w_out": ([D, D], F32),
            "w_router": ([D, 36], F32), "b_router": ([1, 36], F32), "exp_w1": ([NE, D, 512], F32),
            "exp_w3": ([NE, D, 512], F32), "exp_w2": ([NE, 512, D], F32), "ident": ([128, 128], F32),
            "trib": ([128, 128], F32), "sel": ([32, NE * 128], F32),
        }
        got = {}

        def __call__(self, name):
            if name not in self.got:
                shp, dty = self.specs[name]
                self.got[name] = din(name, shp, dty)
            return self.got[name]
    L = _L()
    _L.got = {}
    y_d = dt("y", [nb, S, D], F32, kind="ExternalOutput").ap()
    dbg_d = {}

    with ExitStack() as es:
        kb = KB(nc, es)

        uid = [0]

        def sb(name, shape, dtype, st=None):
            uid[0] += 1
            return (st or es).enter_context(nc.sbuf_tensor("sb%d_%s" % (uid[0], name), list(shape), dtype))

        def dbg_out(name, ap, shape, R, dtype=F32):
            if not dbg:
                return
            d = dt("dbg_" + name, list(shape), dtype, kind="ExternalOutput").ap()
            dbg_d[name] = d
            kb.dma("sp", d, ap, R=R)

        PS = [es.enter_context(nc.psum_tensor("ps%d" % i, [128, 512], F32)) for i in range(8)]
        PT = [T() for _ in range(8)]

        ident_f = sb("ident_f", [128, 128], F32)
        ident_b = sb("ident_b", [128, 128], BF16)
        ident4 = sb("ident4", [128, 4, 128], BF16)
        trib = sb("trib", [128, 128], BF16)
        ones_b = sb("ones_b", [128, 64], BF16)
        ones_f = sb("ones_f", [1, 128], F32)
        bd_ones = sb("bd_ones", [128, 128], BF16)
        pow2h = sb("pow2h", [128, 32], F32)
        gains = sb("gains", [128, 8], F32)
        gq8 = sb("gq8", [128, 4], F32)
        bfgt = sb("bfgt", [8, 1], F32)
        nbfgt = sb("nbfgt", [8, 1], F32)
        bgate = sb("bgate", [128, 16], F32)
        wr_f = sb("wr_f", [128, KC, 36], F32)
        brb = sb("brb", [128, 36], F32)
        wr_hi = sb("wr_hi", [128, KC, 36], BF16)
        wr_lo = sb("wr_lo", [128, KC, 36], BF16)
        cT = T()
        with ExitStack() as ph:
            trib_f = sb("trib_f", [128, 128], F32, ph)
            kb.dma("sp", ident_f[:], L("ident"), W=[cT])
            kb.dma("sp", trib_f[:], L("trib"), W=[cT])
            kb.dma("sp", gains[0:64, :], L("gains"), W=[cT])
            kb.dma("sp", gains[64:128, :], L("gains"), W=[cT])
            kb.dma("sp", bfgt[:], L("b_fgt"), W=[cT])
            kb.dma("sp", bgate[:], L("b_gate"), W=[cT])
            kb.dma("sp", brb[:], L("b_router").partition_broadcast(128), W=[cT])
            kb.dma("sp", wr_f[:], L("w_router").rearrange("(k p) n -> p k n", p=128), W=[cT])
            kb.cp("dve", ident_b[:], ident_f[:], [cT], [cT])
            for j in range(4):
                kb.cp("dve", ident4[:, j, :], ident_f[:], [cT], [cT])
            kb.cp("dve", trib[:], trib_f[:], [cT], [cT])
            kb.memset("dve", ones_b[:], 1.0, [cT])
            kb.memset("dve", ones_f[:], 1.0, [cT])
            kb.memset("dve", bd_ones[:], 0.0, [cT])
            for k_ in range(32):
                kb.memset("dve", pow2h[:, k_:k_ + 1], 2.0 ** (-(k_ + 1)), [cT])
            kb.memset("dve", bd_ones[0:64, 0:64], 1.0, [cT])
            kb.memset("dve", bd_ones[64:128, 64:128], 1.0, [cT])
            kb.ts("dve", gq8[:, 0:1], gains[:, 0:1], 0.125, None, ALU.mult, None, [cT], [cT])
            kb.ts("dve", gq8[:, 1:2], gains[:, 2:3], 0.125, None, ALU.mult, None, [cT], [cT])
            kb.ts("dve", gq8[:, 2:3], gains[:, 4:5], 0.125, None, ALU.mult, None, [cT], [cT])
            kb.ts("dve", nbfgt[:], bfgt[:], -1.0, None, ALU.mult, None, [cT], [cT])
            kb.cp("dve", wr_hi[:], wr_f[:], [cT], [cT])
            kb.tt("dve", wr_f[:], wr_f[:], wr_hi[:], ALU.subtract, [cT], [cT])
            kb.cp("dve", wr_lo[:], wr_f[:], [cT], [cT])
            kb.barrier()

        modb = sb("modb", [128, 6 * D], F32)
        modT = T()
        win_v = L("w_in").rearrange("(k p) n -> p k n", p=128)
        yT = [[T() for _ in range(NT)] for _ in range(nb)]
        done = False

        for b in range(nb):
            with ExitStack() as ph:
                cact = sb("cact", [128, KC], F32, ph)
                crep = sb("crep", [128, KC, 128], F32, ph)
                n1gb = sb("n1gb", [128, D], F32, ph)
                n2gb = sb("n2gb", [128, D], F32, ph)
                awt = [sb("awt%d" % i, [128, KC, 512], F32, ph) for i in range(2)]
                abt = [sb("abt%d" % i, [1, 512], F32, ph) for i in range(2)]
                awT = [T(), T()]
                cTk = T()
                kb.dma("sp", n1gb[:], L("norm1_g").partition_broadcast(128), W=[cTk])
                kb.dma("sp", n2gb[:], L("norm2_g").partition_broadcast(128), W=[cTk])
                kb.dma("sp", cact[:], L("cT")[b], W=[cTk])
                kb.act(cact[:], cact[:], AF.Silu, [cTk], [cTk])
                for k in range(KC):
                    kb.cp("dve", crep[:, k, :], cact[:, k:k + 1].to_broadcast([128, 128]), [cTk], [cTk])
                adaw_v = L("ada_w").rearrange("(k p) n -> p k n", p=128)
                for g in range(12):
                    i = g % 2
                    kb.dma("sp", awt[i][:], adaw_v[:, :, g * 512:(g + 1) * 512], W=[awT[i]])
                    kb.dma("sp", abt[i][:], L("ada_b")[:, g * 512:(g + 1) * 512], W=[awT[i]])
                    pi = g % 2
                    for k in range(KC):
                        kb.mm(PS[pi][:, :], crep[:, k, :], awt[i][:, k, :], k == 0, False, [cTk, awT[i]], [PT[pi]])
                    kb.mm(PS[pi][:, :], ones_f[0:1, :], abt[i][:], False, True, [awT[i], cT], [PT[pi]])
                    kb.cp("act", modb[:, g * 512:(g + 1) * 512], PS[pi][:, :], [PT[pi]], [modT])
                kb.stt("dve", modb[:, D:2 * D], modb[:, D:2 * D], 1.0, n1gb[:], ALU.add, ALU.mult, [modT, cTk], [modT])
                kb.stt("dve", modb[:, 4 * D:5 * D], modb[:, 4 * D:5 * D], 1.0, n2gb[:], ALU.add, ALU.mult, [modT, cTk], [modT])
                if b == 0:
                    dbg_out("mod", modb[0:1, :], [1, 6 * D], [modT])
                kb.barrier()
            if stop == "M":
                break

            with ExitStack() as pb:
                hT = sb("hT", [128, KC, S], BF16, pb)
                hTT = [T() for _ in range(NCH)]
                WTT = sb("WTT", [32, S], F32, pb)
                WTTT = T()
                with ExitStack() as pa:
                    OFT = sb("OFT", [128, 4, S], BF16, pa)
                    ODT = sb("ODT", [128, 4, S], BF16, pa)
                    OFTT, ODTT = T(), T()
                    with ExitStack() as ph:
                        xt = [sb("xt%d" % i, [128, D], F32, ph) for i in range(2)]
                        xtT = [T(), T()]
                        junk = sb("junk", [128, D], BF16, ph)
                        jT = T()
                        st = sb("stat", [128, 4], F32, ph)
                        sT = T()
                        tmp = sb("tmp", [128, D], F32, ph)
                        tmpT = T()
                        hb = [sb("hb%d" % i, [128, D], BF16, ph) for i in range(2)]
                        hbT = [T(), T()]
                        for t in range(NT):
                            i = t % 2
                            kb.dma("sp", xt[i][:], L("x")[b, t * 128:(t + 1) * 128, :], W=[xtT[i]])
                            kb.act(junk[:], xt[i][:], AF.Square, [xtT[i]], [jT, sT], accum=st[:, 0:1])
                            kb.act(st[:, 1:2], st[:, 0:1], AF.Sqrt, [sT], [sT], bias=EPS, scale=1.0 / D)
                            kb.op("dve", lambda e: e.reciprocal(st[:, 2:3], st[:, 1:2]), [sT], [sT])
                            kb.stt("dve", tmp[:], xt[i][:], st[:, 2:3], modb[:, D:2 * D], ALU.mult, ALU.mult,
                                   [xtT[i], sT, modT], [tmpT])
                            kb.tt("pool", hb[i][:], tmp[:], modb[:, 0:D], ALU.add, [tmpT, modT], [hbT[i]])
                            for j in range(2):
                                pv = PS[6 + j][:, 0:256].bitcast(BF16).rearrange("p (a c) -> p a c", a=4)
                                for a in range(4):
                                    k = 4 * j + a
                                    kb.tr(pv[:, a, :], hb[i][:, k * 128:(k + 1) * 128], ident_b[:], [hbT[i], cT], [PT[6 + j]])
                                kb.cp("act" if j == 0 else "dve", hT[:, 4 * j:4 * j + 4, t * 128:(t + 1) * 128], pv,
                                      [PT[6 + j]], [hTT[t // 4]])
                        if b == 0:
                            dbg_out("hT", hT[:, 0, :], [128, S], hTT, BF16)
                        kb.barrier()
                    if stop == "N1":
                        break

                    with ExitStack() as ph:
                        FQT = sb("FQT", [70, 4, S], BF16, ph)
                        FKT = sb("FKT", [70, 4, S], BF16, ph)
                        FV = sb("FV", [128, NT, 512], BF16, ph)
                        G = sb("G", [8, S], F32, ph)
                        FQTT, FKTT, FVT, GT = T(), T(), T(), T()
                        with ExitStack() as p2:
                            wq = [sb("wq%d" % i, [128, KC, 512], BF16, p2) for i in range(1)]
                            wqT = [T()]
                            Gx = sb("Gx", [8, S], F32, p2)
                            wfl = sb("wfl", [128, KC, 8], BF16, p2)
                            kb.dma("pool", wq[0][:], win_v[:, :, C_FV:C_FV + 512], W=[wqT[0]])
                            for t in range(NT):
                                pi = t % 2
                                for k in range(KC):
                                    kb.mm(PS[pi][:, :], hT[:, k, t * 128:(t + 1) * 128], wq[0][:, k, :], k == 0, k == KC - 1,
                                          [wqT[0], hTT[t // 4]], [PT[pi]])
                                kb.cp("act" if pi == 0 else "dve", FV[:, t, :], PS[pi][:, :], [PT[pi]], [FVT])
                            kb.dma("pool", wfl[:], win_v[:, :, C_FL:C_FL + 8], W=[GT])
                            for c in range(NCH):
                                pi = 4 + c % 2
                                for k in range(KC):
                                    kb.mm(PS[pi][0:8, :], wfl[:, k, :], hT[:, k, c * 512:(c + 1) * 512], k == 0, k == KC - 1,
                                          [GT, hTT[c]], [PT[pi]])
                                kb.act(Gx[:, c * 512:(c + 1) * 512], PS[pi][0:8, :], AF.Exp, [PT[pi], cT], [GT], bias=nbfgt[:], scale=-1.0)
                            kb.act(Gx[:], Gx[:], AF.Ln, [GT], [GT], bias=1.0, scale=1.0)
                            kb.op("dve", lambda e: e.tensor_tensor_scan(G[:], Gx[:], Gx[:], 0.0, ALU.add, ALU.max), [GT], [GT])
                            if b == 0:
                                dbg_out("G", G[:], [8, S], [GT])
                            kb.barrier()
                        pbuf = [sb("pbuf%d" % i, [128, 512], BF16, ph) for i in range(3)]
                        pbT = [T() for _ in range(3)]
                        rinv = sb("rinv", [128, 512], F32, ph)
                        rT = T()
                        for hh in range(2):
                            with ExitStack() as p2:
                                wq = [sb("wq%d" % i, [128, KC, 256], BF16, p2) for i in range(2)]
                                wqT = [T(), T()]
                                sq = sb("sq", [64, 512], BF16, p2)
                                sqT = T()
                                rs = sb("rs", [64, 512], F32, p2)
                                rsT = T()
                                Gy = sb("Gy", [8, S], F32, p2)
                                Gs = [sb("Gs%d" % i, [8, S], BF16, p2) for i in range(2)]
                                GsT = T()
                                for qi, (cbase, dst, dstT, gcol) in enumerate(((C_FQ, FQT, FQTT, gq8[0:64, 0:1]), (C_FK, FKT, FKTT, gains[0:64, 1:2]))):
                                    kb.dma("pool", wq[qi][:], win_v[:, :, cbase + hh * 256:cbase + hh * 256 + 256], W=[wqT[qi]])
                                    for h in range(4):
                                        for c in range(NCH):
                                            pi = (h * NCH + c) % 2
                                            for k in range(KC):
                                                kb.mm(PS[pi][0:64, :], wq[qi][:, k, h * 64:(h + 1) * 64], hT[:, k, c * 512:(c + 1) * 512],
                                                      k == 0, k == KC - 1, [wqT[qi], hTT[c]], [PT[pi]])
                                            kb.act(sq[:], PS[pi][0:64, :], AF.Square, [PT[pi]], [sqT])
                                            kb.mm(PS[2 + pi][0:64, :], ones_b[0:64, :], sq[:], True, True, [sqT, cT], [PT[2 + pi]])
                                            kb.act(rs[:], PS[2 + pi][0:64, :], AF.Sqrt, [PT[2 + pi]], [rsT], bias=EPS, scale=1.0 / 64)
                                            kb.op("dve", lambda e: e.reciprocal(rs[:], rs[:]), [rsT], [rsT])
                                            kb.stt("dve", dst[0:64, h, c * 512:(c + 1) * 512], PS[pi][0:64, :], gcol, rs[:],
                                                   ALU.mult, ALU.mult, [PT[pi], rsT, cT], [dstT])
                                kb.memset("dve", FQT[64:70, :, :], 1.0, [FQTT])
                                kb.memset("dve", FKT[64:70, :, :], 1.0, [FKTT])
                                for j in range(3):
                                    src = G if j == 0 else Gy
                                    kb.cp("dve", Gs[0][:], src[:], [GT, GsT], [GsT])
                                    kb.ts("dve", Gs[1][:], Gs[0][:], -1.0, None, ALU.mult, None, [GsT], [GsT])
                                    for h in range(4):
                                        kb.dma("sp", FQT[64 + j:65 + j, h, :], Gs[1][4 * hh + h:4 * hh + h + 1, :], R=[GsT], W=[FQTT])
                                        kb.dma("sp", FKT[67 + j:68 + j, h, :], Gs[0][4 * hh + h:4 * hh + h + 1, :], R=[GsT], W=[FKTT])
                                    if j < 2:
                                        kb.tt("dve", Gy[:], src[:], Gs[0][:], ALU.subtract, [GT, GsT], [GsT])
                                if b == 0 and hh == 0:
                                    dbg_out("FQT0", FQT[:, 0, :], [70, S], [FQTT], BF16)
                                    dbg_out("FKT0", FKT[:, 0, :], [70, S], [FKTT], BF16)
                                kb.barrier()
                            osl = slice(64 * hh, 64 * hh + 64)
                            units = [(h, c, kt) for h in range(4) for c in range(NCH) for kt in range(4 * c + 4)]

                            def fox_s(ui):
                                h, c, kt = units[ui]
                                j = kt - 4 * c
                                c0 = 128 * j if j >= 0 else 0
                                ps = ui % 2
                                kb.mm(PS[ps][:, c0:512], FKT[0:70, h, kt * 128:(kt + 1) * 128],
                                      FQT[0:70, h, c * 512 + c0:(c + 1) * 512], True, j < 0, [FKTT, FQTT], [PT[ps]])
                                if j >= 0:
                                    kb.mm(PS[ps][:, c0:c0 + 128], trib[:], ident_b[:], False, True, [cT], [PT[ps]])
                                kb.act(pbuf[ui % 3][:, c0:512], PS[ps][:, c0:512], AF.Exp, [PT[ps]], [pbT[ui % 3]])

                            def fox_pv(ui):
                                h, c, kt = units[ui]
                                j = kt - 4 * c
                                c0 = 128 * j if j >= 0 else 0
                                nkt = 4 * c + 4
                                po = 2 + (h * NCH + c) % 2
                                pl = 4 + (h * NCH + c) % 2
                                hg = 4 * hh + h
                                pbi = ui % 3
                                kb.mm(PS[po][osl, c0:512], FV[:, kt, hg * 64:(hg + 1) * 64], pbuf[pbi][:, c0:512],
                                      kt == 0, kt == nkt - 1, [FVT, pbT[pbi]], [PT[po]])
                                kb.mm(PS[pl][osl, c0:512], ones_b[:, :], pbuf[pbi][:, c0:512],
                                      kt == 0, kt == nkt - 1, [cT, pbT[pbi]], [PT[pl]])
                                if kt == nkt - 1:
                                    kb.op("dve", lambda e: e.reciprocal(rinv[osl, :], PS[pl][osl, :]), [PT[pl]], [rT])
                                    kb.tt("dve", OFT[osl, h, c * 512:(c + 1) * 512], PS[po][osl, :], rinv[osl, :], ALU.mult,
                                          [PT[po], rT], [OFTT])

                            fox_s(0)
                            for ui in range(len(units)):
                                if ui + 1 < len(units):
                                    fox_s(ui + 1)
                                fox_pv(ui)
                            kb.barrier()
                        if b == 0:
                            dbg_out("OFT", OFT[:], [128, 4, S], [OFTT], BF16)
                        kb.barrier()
                    if stop == "A1":
                        break

                    with ExitStack() as ph:
                        DQT = sb("DQT", [128, 4, S], BF16, ph)
                        DK = [sb("DK%d" % i, [128, S], BF16, ph) for i in range(2)]
                        DV = sb("DV", [128, NT, 64], BF16, ph)
                        IQT = sb("IQT", [128, 4, S], BF16, ph)
                        IK = [sb("IK%d" % i, [128, S], BF16, ph) for i in range(2)]
                        IW = sb("IW", [128, NT, 8], F32, ph)
                        tabT, DQTT, DKTT, DVT, IQTT, IKTT, IWT = T(), T(), T(), T(), T(), T(), T()
                        with ExitStack() as p1:
                            CS = sb("CS", [128, S], F32, p1)
                            SN = sb("SN", [128, S], F32, p1)
                            with ExitStack() as p2:
                                posi = sb("posi", [128, S], I32, p2)
                                ra = sb("ra", [128, S], F32, p2)
                                ua = sb("ua", [128, S], F32, p2)
                                na = sb("na", [128, S], F32, p2)
                                kb.dma("sp", posi[:], L("pos")[b:b + 1, :].partition_broadcast(128), W=[tabT])
                                kb.cp("dve", ra[:], posi[:], [tabT], [tabT])
                                kb.ts("dve", ra[:], ra[:], gains[:, 6:7], 1.0 / TWO_PI, ALU.mult, ALU.mult, [tabT, cT], [tabT])
                                for which, dst in ((0, SN), (1, CS)):
                                    kb.ts("dve", ua[:], ra[:], 0.25 * which, None, ALU.add, None, [tabT], [tabT])
                                    kb.cp("dve", posi[:], ua[:], [tabT], [tabT])
                                    kb.cp("dve", na[:], posi[:], [tabT], [tabT])
                                    kb.tt("dve", ua[:], ua[:], na[:], ALU.subtract, [tabT], [tabT])
                                    kb.stt("dve", na[:], ua[:], 0.5, ua[:], ALU.is_gt, ALU.subtract, [tabT], [tabT])
                                    kb.stt("dve", ua[:], na[:], 0.5, na[:], ALU.is_gt, ALU.subtract, [tabT], [tabT])
                                    kb.act(dst[:], ua[:], AF.Sin, [tabT], [tabT], scale=TWO_PI * (1.0 - 1e-6))
                                kb.ts("dve", SN[:], SN[:], gains[:, 7:8], None, ALU.mult, None, [tabT, cT], [tabT])
                                if b == 0:
                                    dbg_out("CS", CS[0:64, :], [64, S], [tabT])
                                    dbg_out("SN", SN[0:64, :], [64, S], [tabT])
                                kb.barrier()
                            if stop == "T":
                                break
                            wsw_v = L("w_sw").rearrange("(k p) n -> p k n", p=128)
                            with ExitStack() as p2:
                                wA = sb("wA", [128, KC, 512], BF16, p2)
                                wB = sb("wB", [128, KC, 512], BF16, p2)
                                wA1 = sb("wA1", [128, KC, 64], BF16, p2)
                                wB1 = sb("wB1", [128, KC, 64], BF16, p2)
                                wiw = sb("wiw", [128, KC, 8], BF16, p2)
                                wT = T()
                                sq = sb("sq", [128, 512], BF16, p2)
                                rs = sb("rs", [128, 512], F32, p2)
                                t1 = sb("t1", [128, 512], F32, p2)
                                t2 = sb("t2", [128, 512], F32, p2)
                                t3 = sb("t3", [128, 512], BF16, p2)
                                sqT, rsT, t1T, t2T, t3T = T(), T(), T(), T(), T()
                                for i2 in range(2):
                                    kb.memset("pool", DK[i2][:], 0.0, [DKTT])
                                    kb.memset("pool", IK[i2][:], 0.0, [IKTT])

                                def rope_proj(cA, cB, nheads, dst3, dst2, dstT, gA, gB, norm):
                                    ncol = 64 * nheads
                                    wa, wb = (wA, wB) if nheads == 8 else (wA1, wB1)
                                    kb.dma("pool", wa[:, :, 0:ncol], win_v[:, :, cA:cA + ncol], W=[wT])
                                    kb.dma("pool", wb[:, :, 0:ncol], wsw_v[:, :, cB:cB + ncol], W=[wT])
                                    pairs = [(h, h + 4) for h in range(4)] if nheads == 8 else [(0, 0)]
                                    for (hl, hu) in pairs:
                                        for c in range(NCH):
                                            pi = c % 2
                                            csl = slice(c * 512, (c + 1) * 512)
                                            for (hh_, p0) in ((hl, 0), (hu, 64)):
                                                osl = slice(p0, p0 + 64)
                                                for k in range(KC):
                                                    kb.mm(PS[pi][osl, :], wa[:, k, hh_ * 64:(hh_ + 1) * 64], hT[:, k, csl], k == 0, k == KC - 1,
                                                          [wT, hTT[c]], [PT[pi]])
                                                for k in range(KC):
                                                    kb.mm(PS[2 + pi][osl, :], wb[:, k, hh_ * 64:(hh_ + 1) * 64], hT[:, k, csl], k == 0, k == KC - 1,
                                                          [wT, hTT[c]], [PT[2 + pi]])
                                            if norm:
                                                kb.act(sq[:], PS[pi][:, :], AF.Square, [PT[pi]], [sqT])
                                                kb.mm(PS[4 + pi][:, :], bd_ones[:], sq[:], True, True, [sqT, cT], [PT[4 + pi]])
                                                kb.act(rs[:], PS[4 + pi][:, :], AF.Sqrt, [PT[4 + pi]], [rsT], bias=EPS, scale=1.0 / 64)
                                                kb.op("dve", lambda e: e.reciprocal(rs[:], rs[:]), [rsT], [rsT])
                                                kb.stt("dve", t1[:], PS[pi][:, :], gA, rs[:], ALU.mult, ALU.mult, [PT[pi], rsT, cT], [t1T])
                                                kb.stt("dve", t2[:], PS[2 + pi][:, :], gB, rs[:], ALU.mult, ALU.mult, [PT[2 + pi], rsT, cT], [t2T])
                                                kb.tt("pool", t1[:], t1[:], CS[:, csl], ALU.mult, [t1T, tabT], [t1T])
                                                kb.tt("pool", t2[:], t2[:], SN[:, csl], ALU.mult, [t2T, tabT], [t2T])
                                            else:
                                                kb.tt("dve", t1[:], PS[pi][:, :], CS[:, csl], ALU.mult, [PT[pi], tabT], [t1T])
                                                kb.tt("dve", t2[:], PS[2 + pi][:, :], SN[:, csl], ALU.mult, [PT[2 + pi], tabT], [t2T])
                                            if dst3 is not None:
                                                kb.tt("pool", dst3[:, hl, csl], t1[:], t2[:], ALU.add, [t1T, t2T], [dstT])
                                            else:
                                                kb.tt("pool", t3[:], t1[:], t2[:], ALU.add, [t1T, t2T], [t3T])
                                                kb.cp("dve", dst2[0][0:64, csl], t3[0:64, :], [t3T], [dstT])
                                                kb.cp("dve", dst2[1][64:128, csl], t3[64:128, :], [t3T], [dstT])

                                rope_proj(C_DQ, 0, 8, DQT, None, DQTT, gq8[:, 1:2], gq8[:, 2:3], True)
                                rope_proj(C_DK, 512, 1, None, DK, DKTT, gains[:, 3:4], gains[:, 5:6], True)
                                rope_proj(C_IQ, 576, 8, IQT, None, IQTT, None, None, False)
                                rope_proj(C_IK, 1088, 1, None, IK, IKTT, None, None, False)
                                kb.dma("pool", wA1[:], win_v[:, :, C_DV:C_DV + 64], W=[wT])
                                kb.dma("pool", wiw[:], win_v[:, :, C_IW:C_IW + 8], W=[wT])
                                for t in range(NT):
                                    pi = t % 2
                                    for k in range(KC):
                                        kb.mm(PS[pi][:, 0:64], hT[:, k, t * 128:(t + 1) * 128], wA1[:, k, :], k == 0, k == KC - 1,
                                              [wT, hTT[t // 4]], [PT[pi]])
                                    for k in range(KC):
                                        kb.mm(PS[2 + pi][:, 0:8], hT[:, k, t * 128:(t + 1) * 128], wiw[:, k, :], k == 0, k == KC - 1,
                                              [wT, hTT[t // 4]], [PT[2 + pi]])
                                    kb.cp("act", DV[:, t, :], PS[pi][:, 0:64], [PT[pi]], [DVT])
                                    kb.cp("dve", IW[:, t, :], PS[2 + pi][:, 0:8], [PT[2 + pi]], [IWT])
                                if b == 0:
                                    dbg_out("DQT", DQT[:], [128, 4, S], [DQTT], BF16)
                                    dbg_out("DKT", DK[0][:], [128, S], [DKTT], BF16)
                                    dbg_out("IQT", IQT[:], [128, 4, S], [IQTT], BF16)
                                    dbg_out("IKT", IK[1][:], [128, S], [IKTT], BF16)
                                    dbg_out("IW", IW[:], [128, NT, 8], [IWT])
                                kb.barrier()
                        if stop in ("P2", "T"):
                            break
                        scb = [sb("scb%d" % i, [128, S], F32, ph) for i in range(2)]
                        scT = [T(), T()]
                        MB = [sb("MB%d" % i, [128, S], BF16, ph) for i in range(2)]
                        MBT = [T(), T()]
                        junk = sb("junkb", [128, S], BF16, ph)
                        jT = T()
                        wtab = sb("wtab", [128, 32], F32, ph)
                        rbuf = [sb("rbuf%d" % i, [128, 512], F32, ph) for i in range(2)]
                        rbT = [T(), T()]
                        bs = [sb("bs%d" % i, [128, 8], F32, ph) for i in range(2)]
                        bsT = [T(), T()]
                        pbuf = [sb("pbufd%d" % i, [128, 512], BF16, ph) for i in range(3)]
                        pbT = [T() for _ in range(3)]
                        rinv = sb("rinvd", [128, 512], F32, ph)
                        rT = T()
                        ctr = {"u": 0, "ur": 0}

                        def dsa_index(i):
                            end = 128 * (i + 1)
                            sc = scb[i % 2]
                            sT_ = scT[i % 2]
                            qsl = slice(i * 128, (i + 1) * 128)
                            ng = (end + 511) // 512
                            for h in range(8):
                                for g in range(ng):
                                    n = min(512, end - 512 * g)
                                    ps = ctr["ur"] % 2
                                    ri = ctr["ur"] % 2
                                    ctr["ur"] += 1
                                    kb.mm(PS[ps][:, 0:n], IQT[:, h % 4, qsl], IK[h // 4][:, g * 512:g * 512 + n], True, True,
                                          [IQTT, IKTT], [PT[ps]])
                                    kb.act(rbuf[ri][:, 0:n], PS[ps][:, 0:n], AF.Relu, [PT[ps]], [rbT[ri]])
                                    if h == 0:
                                        kb.ts("dve", sc[:, g * 512:g * 512 + n], rbuf[ri][:, 0:n], IW[:, i, 0:1], None, ALU.mult, None,
                                              [rbT[ri], IWT], [sT_])
                                    else:
                                        kb.stt("dve", sc[:, g * 512:g * 512 + n], rbuf[ri][:, 0:n], IW[:, i, h:h + 1],
                                               sc[:, g * 512:g * 512 + n], ALU.mult, ALU.add, [rbT[ri], IWT, sT_], [sT_])
                            bsx = bs[i % 2]
                            bsT_ = bsT[i % 2]
                            if i >= 2:
                                kb.op("dve", lambda e: e.tensor_reduce(bsx[:, 0:1], sc[:, 0:end], AX.X, ALU.max), [sT_], [bsT_])
                                kb.op("dve", lambda e: e.tensor_reduce(bsx[:, 1:2], sc[:, 0:end], AX.X, ALU.min), [sT_], [bsT_])
                                kb.tt("dve", bsx[:, 2:3], bsx[:, 0:1], bsx[:, 1:2], ALU.subtract, [bsT_], [bsT_])
                                kb.memset("dve", sc[0:64, end - 64:end], -1e30, [sT_])
                                kb.ts("dve", wtab[:, 0:N_BISECT + 1], pow2h[:, 0:N_BISECT + 1], bsx[:, 2:3], None, ALU.mult, None, [bsT_, cT], [bsT_])
                                kb.stt("dve", bsx[:, 3:4], bsx[:, 2:3], 0.5, bsx[:, 1:2], ALU.mult, ALU.add, [bsT_], [bsT_])
                                for it in range(N_BISECT):
                                    kb.ts("dve", junk[:, 0:end], sc[:, 0:end], bsx[:, 3:4], None, ALU.is_ge, ALU.add, [sT_, bsT_], [jT, bsT_],
                                          accum=bsx[:, 4:5])
                                    kb.ts("dve", bsx[:, 5:6], bsx[:, 4:5], 256.0, 0.5, ALU.is_ge, ALU.subtract, [bsT_], [bsT_])
                                    kb.stt("dve", bsx[:, 3:4], bsx[:, 5:6], wtab[:, it:it + 1], bsx[:, 3:4], ALU.mult, ALU.add, [bsT_], [bsT_])
                                kb.tt("dve", bsx[:, 1:2], bsx[:, 3:4], wtab[:, N_BISECT:N_BISECT + 1], ALU.subtract, [bsT_], [bsT_])
                            else:
                                kb.memset("dve", bsx[:, 1:2], -1e29, [bsT_])
                                kb.memset("dve", sc[0:64, end - 64:end], -1e30, [sT_])
                            kb.ts("dve", MB[i % 2][:, 0:end], sc[:, 0:end], bsx[:, 1:2], NEG, ALU.is_lt, ALU.mult, [sT_, bsT_], [MBT[i % 2]])

                        def dsa_attn(i):
                            qsl = slice(i * 128, (i + 1) * 128)
                            mb = MB[i % 2]
                            mT_ = MBT[i % 2]
                            aunits = [(hg, kt) for hg in range(2) for kt in range(i + 1)]

                            def a_s(ai):
                                hg, kt = aunits[ai]
                                gu = ctr["u"] + ai
                                ps = 6 + gu % 2
                                ksl = slice(kt * 128, (kt + 1) * 128)
                                psv = PS[ps][:, :].rearrange("p (a c) -> p a c", a=4)
                                kb.mm(psv, DK[hg][:, ksl], DQT[:, :, qsl], True, False, [DKTT, DQTT], [PT[ps]])
                                kb.mm(psv, mb[:, ksl], ident4[:], False, True, [mT_, cT], [PT[ps]])
                                kb.act(pbuf[gu % 3][:], PS[ps][:, :], AF.Exp, [PT[ps]], [pbT[gu % 3]])

                            def a_pv(ai):
                                hg, kt = aunits[ai]
                                gu = ctr["u"] + ai
                                hs = slice(64 * hg, 64 * hg + 64)
                                po = 2 + (2 * i + hg) % 2
                                pl = 4 + (2 * i + hg) % 2
                                pbi = gu % 3
                                kb.mm(PS[po][hs, :], DV[:, kt, :], pbuf[pbi][:], kt == 0, kt == i, [DVT, pbT[pbi]], [PT[po]])
                                kb.mm(PS[pl][hs, :], ones_b[:, :], pbuf[pbi][:], kt == 0, kt == i, [cT, pbT[pbi]], [PT[pl]])
                                if kt == i:
                                    kb.op("dve", lambda e: e.reciprocal(rinv[hs, :], PS[pl][hs, :]), [PT[pl]], [rT])
                                    kb.tt("dve", ODT[hs, :, qsl], PS[po][hs, :].rearrange("p (a c) -> p a c", a=4),
                                          rinv[hs, :].rearrange("p (a c) -> p a c", a=4), ALU.mult, [PT[po], rT], [ODTT])

                            a_s(0)
                            for ai in range(len(aunits)):
                                if ai + 1 < len(aunits):
                                    a_s(ai + 1)
                                a_pv(ai)
                            ctr["u"] += len(aunits)

                        dsa_index(0)
                        for i in range(NT):
                            if i + 1 < NT:
                                dsa_index(i + 1)
                            dsa_attn(i)
                        if b == 0:
                            dbg_out("ODT", ODT[:], [128, 4, S], [ODTT], BF16)
                        kb.barrier()
                    if stop == "A2":
                        break

                    with ExitStack() as ph:
                        MT = sb("MT", [128, KC, S], BF16, ph)
                        MTT = [T() for _ in range(NCH)]
                        with ExitStack() as p2:
                            wpf = sb("wpf", [128, 4, D], BF16, p2)
                            wpd = sb("wpd", [128, 4, D], BF16, p2)
                            wstg = sb("wstg", [128, 4, D], F32, p2)
                            wpT = T()
                            wsT = T()
                            for nm, dstw in (("w_proj_fox", wpf), ("w_proj_dsa", wpd)):
                                wv = L(nm).rearrange("(a q d) n -> a d q n", a=2, q=4)
                                for a in range(2):
                                    kb.dma("sp", wstg[64 * a:64 * a + 64, :, :], wv[a], W=[wsT])
                                kb.cp("pool", dstw[:], wstg[:], [wsT], [wpT])
                            gwf = [sb("gwf%d" % i, [128, KC, 128], BF16, p2) for i in range(2)]
                            gwd = [sb("gwd%d" % i, [128, KC, 128], BF16, p2) for i in range(2)]
                            gwT = [T(), T()]
                            sf = [sb("sf%d" % i, [128, 512], F32, p2) for i in range(2)]
                            sd = [sb("sd%d" % i, [128, 512], F32, p2) for i in range(2)]
                            sfT = [T(), T()]
                            sdT = [T(), T()]
                            for cc in range(KC):
                                i = cc % 2
                                kb.dma("pool", gwf[i][:], win_v[:, :, C_GF + cc * 128:C_GF + (cc + 1) * 128], W=[gwT[i]])
                                kb.dma("pool", gwd[i][:], win_v[:, :, C_GD + cc * 128:C_GD + (cc + 1) * 128], W=[gwT[i]])
                                for c in range(NCH):
                                    csl = slice(c * 512, (c + 1) * 512)
                                    par = (cc * NCH + c) % 2
                                    b0 = 4 * par
                                    for q in range(4):
                                        kb.mm(PS[b0][:, :], wpf[:, q, cc * 128:(cc + 1) * 128], OFT[:, q, csl], q == 0, q == 3,
                                              [wpT, OFTT], [PT[b0]])
                                    for q in range(4):
                                        kb.mm(PS[b0 + 1][:, :], wpd[:, q, cc * 128:(cc + 1) * 128], ODT[:, q, csl], q == 0, q == 3,
                                              [wpT, ODTT], [PT[b0 + 1]])
                                    for k in range(KC):
                                        kb.mm(PS[b0 + 2][:, :], gwf[i][:, k, :], hT[:, k, csl], k == 0, k == KC - 1,
                                              [gwT[i], hTT[c]], [PT[b0 + 2]])
                                    for k in range(KC):
                                        kb.mm(PS[b0 + 3][:, :], gwd[i][:, k, :], hT[:, k, csl], k == 0, k == KC - 1,
                                              [gwT[i], hTT[c]], [PT[b0 + 3]])
                                    kb.act(sf[par][:], PS[b0 + 2][:, :], AF.Sigmoid, [PT[b0 + 2], cT], [sfT[par]], bias=bgate[:, cc:cc + 1])
                                    kb.act(sd[par][:], PS[b0 + 3][:, :], AF.Sigmoid, [PT[b0 + 3], cT], [sdT[par]], bias=bgate[:, 8 + cc:9 + cc])
                                    kb.tt("dve", sf[par][:], sf[par][:], PS[b0][:, :], ALU.mult, [sfT[par], PT[b0]], [sfT[par]])
                                    kb.tt("dve", sd[par][:], sd[par][:], PS[b0 + 1][:, :], ALU.mult, [sdT[par], PT[b0 + 1]], [sdT[par]])
                                    kb.tt("pool", MT[:, cc, csl], sf[par][:], sd[par][:], ALU.add, [sfT[par], sdT[par]], [MTT[c]])
                            if b == 0:
                                dbg_out("MT", MT[:, 0, :], [128, S], MTT, BF16)
                            kb.barrier()
                        if stop == "G":
                            break
                        with ExitStack() as p2:
                            wo_b = sb("wo_b", [128, KC, D], BF16, p2)
                            woT = T()
                            kb.dma("pool", wo_b[:], L("w_out").rearrange("(k p) n -> p k n", p=128), W=[woT])
                            xt = [sb("xo%d" % i, [128, D], F32, p2) for i in range(2)]
                            xtT = [T(), T()]
                            tmp = sb("tmpo", [128, D], F32, p2)
                            tmpT = T()
                            junk = sb("junko", [128, D], BF16, p2)
                            jT = T()
                            h2f = sb("h2f", [128, D], F32, p2)
                            h2fT = T()
                            h2hi = sb("h2hi", [128, D], BF16, p2)
                            h2lo = sb("h2lo", [128, D], BF16, p2)
                            loTt = sb("loTt", [128, KC, 128], BF16, p2)
                            hiT, loT, loTT = T(), T(), T()
                            st = sb("stato", [128, 4], F32, p2)
                            sT = T()
                            rt = sb("rt", [128, 160], F32, p2)
                            rtT = T()
                            for t in range(NT):
                                i = t % 2
                                tsl = slice(t * 128, (t + 1) * 128)
                                for half in range(2):
                                    for cc in range(KC):
                                        kb.mm(PS[half][:, :], MT[:, cc, tsl], wo_b[:, cc, half * 512:(half + 1) * 512], cc == 0, cc == KC - 1,
                                              [MTT[t // 4], woT], [PT[half]])
                                kb.dma("sp", xt[i][:], L("x")[b, tsl, :], W=[xtT[i]])
                                for half in range(2):
                                    hsl = slice(half * 512, (half + 1) * 512)
                                    kb.tt("dve", tmp[:, hsl], PS[half][:, :], modb[:, 2 * D + half * 512:2 * D + (half + 1) * 512], ALU.mult,
                                          [PT[half], modT], [tmpT])
                                kb.tt("pool", xt[i][:], xt[i][:], tmp[:], ALU.add, [xtT[i], tmpT], [xtT[i]])
                                kb.dma("sp", y_d[b, tsl, :], xt[i][:], R=[xtT[i]], W=[yT[b][t]])
                                if stop == "O1":
                                    continue
                                kb.act(junk[:], xt[i][:], AF.Square, [xtT[i]], [jT, sT], accum=st[:, 0:1])
                                kb.act(st[:, 1:2], st[:, 0:1], AF.Sqrt, [sT], [sT], bias=EPS, scale=1.0 / D)
                                kb.op("dve", lambda e: e.reciprocal(st[:, 2:3], st[:, 1:2]), [sT], [sT])
                                kb.stt("dve", tmp[:], xt[i][:], st[:, 2:3], modb[:, 4 * D:5 * D], ALU.mult, ALU.mult,
                                       [xtT[i], sT, modT], [tmpT])
                                kb.tt("pool", h2f[:], tmp[:], modb[:, 3 * D:4 * D], ALU.add, [tmpT, modT], [h2fT])
                                kb.cp("dve", h2hi[:], h2f[:], [h2fT], [hiT])
                                kb.tt("pool", h2lo[:], h2f[:], h2hi[:], ALU.subtract, [h2fT, hiT], [loT])
                                for j in range(2):
                                    pv = PS[2 + j][:, 0:256].bitcast(BF16).rearrange("p (a c) -> p a c", a=4)
                                    for a in range(4):
                                        k = 4 * j + a
                                        kb.tr(pv[:, a, :], h2hi[:, k * 128:(k + 1) * 128], ident_b[:], [hiT, cT], [PT[2 + j]])
                                    kb.cp("act" if j == 0 else "dve", hT[:, 4 * j:4 * j + 4, tsl], pv, [PT[2 + j]], [hTT[t // 4]])
                                for j in range(2):
                                    pv = PS[6 + j][:, 0:256].bitcast(BF16).rearrange("p (a c) -> p a c", a=4)
                                    for a in range(4):
                                        k = 4 * j + a
                                        kb.tr(pv[:, a, :], h2lo[:, k * 128:(k + 1) * 128], ident_b[:], [loT, cT], [PT[6 + j]])
                                    kb.cp("act" if j == 0 else "dve", loTt[:, 4 * j:4 * j + 4, :], pv, [PT[6 + j]], [loTT])
                                if stop == "O2":
                                    continue
                                nmm = 3 * KC
                                q = 0
                                for (lh, lhT_, wgt) in ((hT, hTT[t // 4], wr_hi), (hT, hTT[t // 4], wr_lo), (loTt, loTT, wr_hi)):
                                    for k in range(KC):
                                        lhs_ap = lh[:, k, tsl] if lh is hT else lh[:, k, :]
                                        kb.mm(PS[4][:, 0:36], lhs_ap, wgt[:, k, :], q == 0, q == nmm - 1, [lhT_, cT], [PT[4]])
                                        q += 1
                                if stop == "O3":
                                    continue
                                R_ = [rtT]
                                lg = rt[:, 0:36]
                                kb.tt("dve", lg, PS[4][:, 0:36], brb[:], ALU.add, [PT[4], cT], R_)
                                kb.op("dve", lambda e: e.tensor_reduce(rt[:, 40:41], rt[:, 0:4], AX.X, ALU.max), R_, R_)
                                kb.ts("dve", rt[:, 44:48], rt[:, 0:4], rt[:, 40:41], None, ALU.is_ge, None, R_, R_)
                                kb.ts("dve", rt[:, 41:42], rt[:, 40:41], -1.0, None, ALU.mult, None, R_, R_)
                                kb.act(rt[:, 48:52], rt[:, 0:4], AF.Exp, R_, R_, bias=rt[:, 41:42], accum=rt[:, 42:43])
                                kb.op("dve", lambda e: e.reciprocal(rt[:, 43:44], rt[:, 42:43]), R_, R_)
                                kb.ts("dve", rt[:, 52:56], rt[:, 44:48], -1.0, 1e30, ALU.add, ALU.mult, R_, R_)
                                for gi in range(4):
                                    kb.ts("dve", rt[:, 60 + 8 * gi:68 + 8 * gi], rt[:, 4 + 8 * gi:12 + 8 * gi], rt[:, 44 + gi:45 + gi],
                                          rt[:, 52 + gi:53 + gi], ALU.mult, ALU.add, R_, R_)
                                lem = rt[:, 60:92]
                                kb.op("dve", lambda e: e.tensor_reduce(rt[:, 56:57], rt[:, 60:92], AX.X, ALU.max), R_, R_)
                                kb.ts("dve", rt[:, 92:124], lem, rt[:, 56:57], None, ALU.is_ge, None, R_, R_)
                                kb.stt("dve", lem, rt[:, 92:124], -1e30, lem, ALU.mult, ALU.add, R_, R_)
                                kb.op("dve", lambda e: e.tensor_reduce(rt[:, 57:58], rt[:, 60:92], AX.X, ALU.max), R_, R_)
                                kb.ts("dve", rt[:, 124:156], lem, rt[:, 57:58], None, ALU.is_ge, None, R_, R_)
                                kb.tt("dve", rt[:, 58:59], rt[:, 57:58], rt[:, 56:57], ALU.subtract, R_, R_)
                                kb.act(rt[:, 59:60], rt[:, 58:59], AF.Exp, R_, R_)
                                kb.ts("dve", rt[:, 36:37], rt[:, 59:60], 1.0, None, ALU.add, None, R_, R_)
                                kb.op("dve", lambda e: e.reciprocal(rt[:, 36:37], rt[:, 36:37]), R_, R_)
                                kb.tt("dve", rt[:, 37:38], rt[:, 36:37], rt[:, 43:44], ALU.mult, R_, R_)
                                kb.tt("dve", rt[:, 38:39], rt[:, 37:38], rt[:, 59:60], ALU.mult, R_, R_)
                                kb.ts("dve", rt[:, 92:124], rt[:, 92:124], rt[:, 37:38], None, ALU.mult, None, R_, R_)
                                kb.stt("dve", rt[:, 92:124], rt[:, 124:156], rt[:, 38:39], rt[:, 92:124], ALU.mult, ALU.add, R_, R_)
                                if stop == "O4":
                                    continue
                                kb.mm(PS[5][0:32, 0:128], rt[:, 92:124], ident_f[:], True, True, [rtT, cT], [PT[5]])
                                kb.cp("act", WTT[:, tsl], PS[5][0:32, 0:128], [PT[5]], [WTTT])
                            if b == 0:
                                dbg_out("WTT", WTT[:], [32, S], [WTTT])
                                dbg_out("h2T", hT[:, 0, :], [128, S], hTT, BF16)
                            kb.barrier()
                if stop in ("O", "O1", "O2", "O3", "O4"):
                    break
                with ExitStack() as ph:
                    XM = sb("XM", [128, NT, D], F32, ph)
                    XMT = [T() for _ in range(NT)]
                    W1 = [sb("W1_%d" % i, [128, KC, 512], BF16, ph) for i in range(2)]
                    W3 = [sb("W3_%d" % i, [128, KC, 512], BF16, ph) for i in range(2)]
                    W2 = [sb("W2_%d" % i, [128, 4, D], BF16, ph) for i in range(2)]
                    wT = [T(), T()]
                    w2T = [T(), T()]
                    gT = [sb("gT%d" % i, [128, 4, 512], BF16, ph) for i in range(2)]
                    gTT = [T(), T()]
                    sa = [sb("sa%d" % i, [128, 512], F32, ph) for i in range(2)]
                    saT = [T(), T()]
                    g1 = [sb("g1%d" % i, [128, 512], BF16, ph) for i in range(2)]
                    g1T = [T(), T()]
                    wtb = [sb("wtb%d" % i, [128, 512], F32, ph) for i in range(2)]
                    wtbT = [T(), T()]
                    selE = [sb("selE%d" % i, [32, 128], F32, ph) for i in range(2)]
                    selT = [T(), T()]
                    xo = sb("xfin", [128, D], F32, ph)
                    xoT = T()
                    ne_run = NE if stop != "E1" else 2

                    def moe_load(e):
                        i = e % 2
                        kb.dma("pool", W1[i][:], L("exp_w1")[e].rearrange("(k p) f -> p k f", p=128), W=[wT[i]])
                        kb.dma("pool", W3[i][:], L("exp_w3")[e].rearrange("(k p) f -> p k f", p=128), W=[wT[i]])
                        kb.dma("pool", W2[i][:], L("exp_w2")[e].rearrange("(k p) n -> p k n", p=128), W=[w2T[i]])

                    def moe_scale(e):
                        i = e % 2
                        for fc in range(4):
                            kb.tt("pool", W2[i][:, fc, :], W2[i][:, fc, :], modb[:, 5 * D:6 * D], ALU.mult, [w2T[i], modT], [w2T[i]])
                        kb.cp("dve", selE[i][:], ident_f[0:32, e:e + 1].to_broadcast([32, 128]), [cT], [selT[i]])

                    munits = [(e, c) for e in range(ne_run) for c in range(NCH)]

                    def moe_ab(ui):
                        e, c = munits[ui]
                        i = e % 2
                        csl = slice(c * 512, (c + 1) * 512)
                        par = ui % 2
                        kb.mm(PS[6][:, :], selE[i][:], WTT[:, csl], True, True, [selT[i], WTTT], [PT[6]])
                        kb.cp("act", wtb[par][:], PS[6][:, :], [PT[6]], [wtbT[par]])
                        for fc in range(4):
                            q = fc % 2
                            fsl = slice(fc * 128, (fc + 1) * 128)
                            for k in range(KC):
                                kb.mm(PS[2 * q][:, :], W1[i][:, k, fsl], hT[:, k, csl], k == 0, k == KC - 1, [wT[i], hTT[c]], [PT[2 * q]])
                            for k in range(KC):
                                kb.mm(PS[2 * q + 1][:, :], W3[i][:, k, fsl], hT[:, k, csl], k == 0, k == KC - 1, [wT[i], hTT[c]], [PT[2 * q + 1]])
                            kb.act(sa[q][:], PS[2 * q][:, :], AF.Silu, [PT[2 * q]], [saT[q]])
                            kb.tt("dve", g1[q][:], sa[q][:], PS[2 * q + 1][:, :], ALU.mult, [saT[q], PT[2 * q + 1]], [g1T[q]])
                            kb.tt("pool", gT[par][:, fc, :], g1[q][:], wtb[par][:], ALU.mult, [g1T[q], wtbT[par]], [gTT[par]])

                    def moe_y(ui):
                        e, c = munits[ui]
                        i = e % 2
                        par = ui % 2
                        for tq in range(4):
                            t = 4 * c + tq
                            for half in range(2):
                                py = 4 + (2 * tq + half) % 2
                                hsl = slice(half * 512, (half + 1) * 512)
                                for fc in range(4):
                                    kb.mm(PS[py][:, :], gT[par][:, fc, tq * 128:(tq + 1) * 128], W2[i][:, fc, hsl], fc == 0, fc == 3,
                                          [gTT[par], w2T[i]], [PT[py]])
                                if e == 0:
                                    kb.cp("dve", XM[:, t, hsl], PS[py][:, :], [PT[py]], [XMT[t]])
                                else:
                                    kb.tt("dve", XM[:, t, hsl], XM[:, t, hsl], PS[py][:, :], ALU.add, [XMT[t], PT[py]], [XMT[t]])

                    moe_load(0)
                    moe_scale(0)
                    for ui in range(len(munits)):
                        e, c = munits[ui]
                        moe_ab(ui)
                        if ui >= 1:
                            moe_y(ui - 1)
                        if c == 0 and e + 1 < ne_run:
                            moe_load(e + 1)
                        if c == NCH - 1 and e + 1 < ne_run:
                            moe_scale(e + 1)
                    moe_y(len(munits) - 1)
                    for t in range(NT):
                        tsl = slice(t * 128, (t + 1) * 128)
                        kb.dma("sp", xo[:], y_d[b, tsl, :], R=[yT[b][t]], W=[xoT])
                        kb.tt("pool", xo[:], xo[:], XM[:, t, :], ALU.add, [xoT, XMT[t]], [xoT])
                        kb.dma("sp", y_d[b, tsl, :], xo[:], R=[xoT], W=[yT[b][t]])
                    kb.barrier()
        kb.barrier(("sp",))
    return nc, (dbg_d, set(_L.got))


def _consts():
    ident = np.eye(128, dtype=np.float32)
    q = np.arange(128)[:, None]
    k = np.arange(128)[None, :]
    trib = np.where(k > q, NEG, 0.0).astype(np.float32)
    sel = np.zeros((32, NE, 128), np.float32)
    for e in range(NE):
        sel[e, e, :] = 1.0
    half = 32
    inv = (np.float32(10000.0) ** (-np.arange(half, dtype=np.float32) / np.float32(half))).astype(np.float32)
    invf = np.concatenate([inv, inv]).astype(np.float32)
    sgn = np.concatenate([-np.ones(32, np.float32), np.ones(32, np.float32)])
    return ident, trib, sel.reshape(32, NE * 128), invf, sgn


def _swap_cols(w, base, nheads):
    cols = []
    for h in range(nheads):
        cols.append(w[:, base + h * 64 + 32: base + h * 64 + 64])
        cols.append(w[:, base + h * 64: base + h * 64 + 32])
    return np.concatenate(cols, axis=1)


def prep_shared(inp):
    f = lambda a: np.ascontiguousarray(np.asarray(a, dtype=np.float32))
    ident, trib, sel, invf, sgn = _consts()
    w_in = f(inp["w_in"][0])
    w_sw = np.concatenate([_swap_cols(w_in, C_DQ, 8), _swap_cols(w_in, C_DK, 1),
                           _swap_cols(w_in, C_IQ, 8), _swap_cols(w_in, C_IK, 1)], axis=1)
    sw = lambda g: np.concatenate([g[32:], g[:32]])
    qd, kd = f(inp["qn_dsa"][0]), f(inp["kn_dsa"][0])
    gains = np.stack([f(inp["qn_fox"][0]), f(inp["kn_fox"][0]), qd, kd, sw(qd), sw(kd), invf, sgn], axis=1)
    sh = {
        "ada_w": f(inp["ada_w"][0]), "ada_b": f(inp["ada_b"][0]).reshape(1, -1),
        "norm1_g": f(inp["norm1_g"][0]).reshape(1, -1), "norm2_g": f(inp["norm2_g"][0]).reshape(1, -1),
        "w_in": w_in, "w_sw": f(w_sw), "b_fgt": f(inp["b_fgt"][0]).reshape(8, 1),
        "b_gate": f(f(inp["b_gate"][0]).reshape(16, 128).T), "gains": f(gains),
        "w_proj_fox": f(inp["w_proj_fox"][0]), "w_proj_dsa": f(inp["w_proj_dsa"][0]), "w_out": f(inp["w_out"][0]),
        "w_router": f(np.concatenate([inp["router_w_grp"][0], inp["router_w_exp"][0]], axis=1)),
        "b_router": f(np.concatenate([inp["router_b_grp"][0], inp["router_b_exp"][0]])).reshape(1, 36),
        "exp_w1": f(inp["exp_w1"][0]), "exp_w3": f(inp["exp_w3"][0]), "exp_w2": f(inp["exp_w2"][0]),
        "ident": ident, "trib": trib, "sel": sel,
    }
    return sh


def prep_core(inp, b0, nb):
    x = np.ascontiguousarray(np.asarray(inp["x"][b0:b0 + nb], dtype=np.float32))
    c = np.asarray(inp["c"][b0:b0 + nb], dtype=np.float32)
    cT = np.ascontiguousarray(c.reshape(nb, KC, 128).transpose(0, 2, 1))
    pos = np.ascontiguousarray(np.asarray(inp["positions"][b0:b0 + nb], dtype=np.int32))
    return {"x": x, "cT": cT, "pos": pos}


def kernel(**inputs):
    n = 8
    nb = 4
    nc, (_, used) = build(nb)
    sh = prep_shared(inputs)
    in_maps = []
    for i in range(n):
        m = dict(sh)
        m.update(prep_core(inputs, i * nb, nb))
        in_maps.append({k: v for k, v in m.items() if k in used})
    res = run_bass_kernel_spmd(nc, in_maps, core_ids=list(range(n)))
    return np.concatenate([r["y"] for r in res.results], axis=0).astype(np.float32)
```

```python
import numpy as np
from contextlib import ExitStack
import concourse.bass as bass
import concourse.mybir as mybir
from concourse.bass_utils import run_bass_kernel_spmd

F32 = mybir.dt.float32
BF16 = mybir.dt.bfloat16
I32 = mybir.dt.int32
ALU = mybir.AluOpType
AF = mybir.ActivationFunctionType
AX = mybir.AxisListType

S = 2048
D = 1024
NT = 16
NCH = 4
KC = 8
NE = 32
C_FQ, C_FK, C_FV, C_FL, C_DQ, C_DK, C_DV, C_IQ, C_IK, C_IW, C_GF, C_GD = (
    0, 512, 1024, 1536, 1544, 2056, 2120, 2184, 2696, 2760, 2768, 3792)
IN_COLS = 4816
EPS = 1e-6
NEG = -30000.0
N_BISECT = 22
TWO_PI = 6.283185307179586


class T:
    __slots__ = ("w", "r")

    def __init__(self):
        self.w = {}
        self.r = {}


class KB:
    def __init__(self, nc, es, n_sp=12, n_pool=6):
        self.nc = nc
        self.E = {"pe": nc.tensor, "act": nc.scalar, "dve": nc.vector, "pool": nc.gpsimd, "sp": nc.sync}
        self.semobj = {}
        self.cnt = {}
        for e in ("pe", "act", "dve", "pool"):
            self.semobj[e] = es.enter_context(nc.semaphore("s_" + e))
            self.cnt[e] = 0
        self.known = {e: {} for e in self.E}
        self.dsems = {"sp": [], "pool": []}
        self.dtot = {}
        self.dnext = {"sp": 0, "pool": 0}
        for q, n in (("sp", n_sp), ("pool", n_pool)):
            for i in range(n):
                name = "d_%s%d" % (q, i)
                self.semobj[name] = es.enter_context(nc.semaphore(name))
                self.dsems[q].append(name)
                self.dtot[name] = 0

    @staticmethod
    def _deps(R, W):
        deps = {}
        for t in R:
            for k, v in t.w.items():
                if deps.get(k, 0) < v:
                    deps[k] = v
        for t in W:
            for d in (t.w, t.r):
                for k, v in d.items():
                    if deps.get(k, 0) < v:
                        deps[k] = v
        return deps

    def _wait(self, eng, deps):
        kn = self.known[eng]
        for k, v in deps.items():
            if eng == "pe" and k == "pe":
                continue
            if kn.get(k, 0) >= v:
                continue
            self.E[eng].wait_ge(self.semobj[k], v)
            kn[k] = v

    def op(self, eng, fn, R=(), W=()):
        self._wait(eng, self._deps(R, W))
        ins = fn(self.E[eng])
        self.cnt[eng] += 1
        c = self.cnt[eng]
        ins.then_inc(self.semobj[eng], 1)
        for t in W:
            t.w = {eng: c}
            t.r = {}
        for t in R:
            if t.r.get(eng, 0) < c:
                t.r[eng] = c

    def dma(self, q, out, in_, R=(), W=()):
        deps = self._deps(R, W)
        i = self.dnext[q]
        self.dnext[q] = (i + 1) % len(self.dsems[q])
        name = self.dsems[q][i]
        tot = self.dtot[name]
        if deps.get(name, 0) < tot:
            deps[name] = tot
        self._wait(q, deps)
        self.E[q].dma_start(out=out, in_=in_).then_inc(self.semobj[name], 16)
        tot += 16
        self.dtot[name] = tot
        for t in W:
            t.w = {name: tot}
            t.r = {}
        for t in R:
            t.r[name] = tot

    def barrier(self, engs=("pe", "act", "dve", "pool", "sp")):
        allv = dict(self.cnt)
        allv.update(self.dtot)
        allv = {k: v for k, v in allv.items() if v > 0}
        for e in engs:
            self._wait(e, dict(allv))

    def mm(self, out, lhsT, rhs, start, stop, R, W):
        self.op("pe", lambda e: e.matmul(out, lhsT, rhs, start=start, stop=stop), R, W)

    def tr(self, out, in_, ident, R, W):
        self.op("pe", lambda e: e.transpose(out, in_, ident), R, W)

    def act(self, out, in_, func, R, W, bias=0.0, scale=1.0, accum=None):
        if accum is None:
            self.op("act", lambda e: e.activation(out, in_, func, bias=bias, scale=scale), R, W)
        else:
            self.op("act", lambda e: e.activation(out, in_, func, bias=bias, scale=scale, accum_out=accum), R, W)

    def ts(self, eng, out, in0, s1, s2, op0, op1, R, W, accum=None):
        if accum is None:
            if op1 is None:
                self.op(eng, lambda e: e.tensor_scalar(out, in0, s1, None, op0), R, W)
            else:
                self.op(eng, lambda e: e.tensor_scalar(out, in0, s1, s2, op0, op1), R, W)
        else:
            self.op(eng, lambda e: e.tensor_scalar(out, in0, s1, s2, op0, op1, accum_out=accum), R, W)

    def tt(self, eng, out, in0, in1, op, R, W):
        self.op(eng, lambda e: e.tensor_tensor(out, in0, in1, op), R, W)

    def stt(self, eng, out, in0, scalar, in1, op0, op1, R, W):
        self.op(eng, lambda e: e.scalar_tensor_tensor(out, in0, scalar, in1, op0, op1), R, W)

    def cp(self, eng, out, in_, R, W):
        if eng == "act":
            self.op("act", lambda e: e.copy(out, in_), R, W)
        else:
            self.op(eng, lambda e: e.tensor_copy(out, in_), R, W)

    def memset(self, eng, ap, v, W):
        self.op(eng, lambda e: e.memset(ap, v), (), W)


def build(nb, stop=None, dbg=False):
    nc = bass.Bass("TRN2", target_bir_lowering=False)
    dt = nc.dram_tensor

    def din(name, shape, dtype=F32):
        return dt(name, list(shape), dtype, kind="ExternalInput").ap()

    class _L:
        specs = {
            "x": ([nb, S, D], F32), "cT": ([nb, 128, KC], F32), "pos": ([nb, S], I32),
            "ada_w": ([D, 6 * D], F32), "ada_b": ([1, 6 * D], F32), "norm1_g": ([1, D], F32), "norm2_g": ([1, D], F32),
            "w_in": ([D, IN_COLS], F32), "w_sw": ([D, 1152], F32), "b_fgt": ([8, 1], F32), "b_gate": ([128, 16], F32),
            "gains": ([64, 8], F32), "w_proj_fox": ([512, D], F32), "w_proj_dsa": ([512, D], F32), "w_out": ([D, D], F32),
            "w_router": ([D, 36], F32), "b_router": ([1, 36], F32), "exp_w1": ([NE, D, 512], F32),
            "exp_w3": ([NE, D, 512], F32), "exp_w2": ([NE, 512, D], F32), "ident": ([128, 128], F32),
            "trib": ([128, 128], F32), "sel": ([32, NE * 128], F32),
        }
        got = {}

        def __call__(self, name):
            if name not in self.got:
                shp, dty = self.specs[name]
                self.got[name] = din(name, shp, dty)
            return self.got[name]
    L = _L()
    _L.got = {}
    y_d = dt("y", [nb, S, D], F32, kind="ExternalOutput").ap()
    dbg_d = {}

    with ExitStack() as es:
        kb = KB(nc, es)

        uid = [0]

        def sb(name, shape, dtype, st=None):
            uid[0] += 1
            return (st or es).enter_context(nc.sbuf_tensor("sb%d_%s" % (uid[0], name), list(shape), dtype))

        def dbg_out(name, ap, shape, R, dtype=F32):
            if not dbg:
                return
            d = dt("dbg_" + name, list(shape), dtype, kind="ExternalOutput").ap()
            dbg_d[name] = d
            kb.dma("sp", d, ap, R=R)

        PS = [es.enter_context(nc.psum_tensor("ps%d" % i, [128, 512], F32)) for i in range(8)]
        PT = [T() for _ in range(8)]

        ident_f = sb("ident_f", [128, 128], F32)
        ident_b = sb("ident_b", [128, 128], BF16)
        ident4 = sb("ident4", [128, 4, 128], BF16)
        trib = sb("trib", [128, 128], BF16)
        ones_b = sb("ones_b", [128, 64], BF16)
        ones_f = sb("ones_f", [1, 128], F32)
        bd_ones = sb("bd_ones", [128, 128], BF16)
        gains = sb("gains", [128, 8], F32)
        gq8 = sb("gq8", [128, 4], F32)
        bfgt = sb("bfgt", [8, 1], F32)
        nbfgt = sb("nbfgt", [8, 1], F32)
        bgate = sb("bgate", [128, 16], F32)
        wr_f = sb("wr_f", [128, KC, 36], F32)
        brb = sb("brb", [128, 36], F32)
        wr_hi = sb("wr_hi", [128, KC, 36], BF16)
        wr_lo = sb("wr_lo", [128, KC, 36], BF16)
        cT = T()
        with ExitStack() as ph:
            trib_f = sb("trib_f", [128, 128], F32, ph)
            kb.dma("sp", ident_f[:], L("ident"), W=[cT])
            kb.dma("sp", trib_f[:], L("trib"), W=[cT])
            kb.dma("sp", gains[0:64, :], L("gains"), W=[cT])
            kb.dma("sp", gains[64:128, :], L("gains"), W=[cT])
            kb.dma("sp", bfgt[:], L("b_fgt"), W=[cT])
            kb.dma("sp", bgate[:], L("b_gate"), W=[cT])
            kb.dma("sp", brb[:], L("b_router").partition_broadcast(128), W=[cT])
            kb.dma("sp", wr_f[:], L("w_router").rearrange("(k p) n -> p k n", p=128), W=[cT])
            kb.cp("dve", ident_b[:], ident_f[:], [cT], [cT])
            for j in range(4):
                kb.cp("dve", ident4[:, j, :], ident_f[:], [cT], [cT])
            kb.cp("dve", trib[:], trib_f[:], [cT], [cT])
            kb.memset("dve", ones_b[:], 1.0, [cT])
            kb.memset("dve", ones_f[:], 1.0, [cT])
            kb.memset("dve", bd_ones[:], 0.0, [cT])
            kb.memset("dve", bd_ones[0:64, 0:64], 1.0, [cT])
            kb.memset("dve", bd_ones[64:128, 64:128], 1.0, [cT])
            kb.ts("dve", gq8[:, 0:1], gains[:, 0:1], 0.125, None, ALU.mult, None, [cT], [cT])
            kb.ts("dve", gq8[:, 1:2], gains[:, 2:3], 0.125, None, ALU.mult, None, [cT], [cT])
            kb.ts("dve", gq8[:, 2:3], gains[:, 4:5], 0.125, None, ALU.mult, None, [cT], [cT])
            kb.ts("dve", nbfgt[:], bfgt[:], -1.0, None, ALU.mult, None, [cT], [cT])
            kb.cp("dve", wr_hi[:], wr_f[:], [cT], [cT])
            kb.tt("dve", wr_f[:], wr_f[:], wr_hi[:], ALU.subtract, [cT], [cT])
            kb.cp("dve", wr_lo[:], wr_f[:], [cT], [cT])
            kb.barrier()

        modb = sb("modb", [128, 6 * D], F32)
        modT = T()
        win_v = L("w_in").rearrange("(k p) n -> p k n", p=128)
        yT = [[T() for _ in range(NT)] for _ in range(nb)]
        done = False

        for b in range(nb):
            with ExitStack() as ph:
                cact = sb("cact", [128, KC], F32, ph)
                crep = sb("crep", [128, KC, 128], F32, ph)
                n1gb = sb("n1gb", [128, D], F32, ph)
                n2gb = sb("n2gb", [128, D], F32, ph)
                awt = [sb("awt%d" % i, [128, KC, 512], F32, ph) for i in range(2)]
                abt = [sb("abt%d" % i, [1, 512], F32, ph) for i in range(2)]
                awT = [T(), T()]
                cTk = T()
                kb.dma("sp", n1gb[:], L("norm1_g").partition_broadcast(128), W=[cTk])
                kb.dma("sp", n2gb[:], L("norm2_g").partition_broadcast(128), W=[cTk])
                kb.dma("sp", cact[:], L("cT")[b], W=[cTk])
                kb.act(cact[:], cact[:], AF.Silu, [cTk], [cTk])
                for k in range(KC):
                    kb.cp("dve", crep[:, k, :], cact[:, k:k + 1].to_broadcast([128, 128]), [cTk], [cTk])
                adaw_v = L("ada_w").rearrange("(k p) n -> p k n", p=128)
                for g in range(12):
                    i = g % 2
                    kb.dma("sp", awt[i][:], adaw_v[:, :, g * 512:(g + 1) * 512], W=[awT[i]])
                    kb.dma("sp", abt[i][:], L("ada_b")[:, g * 512:(g + 1) * 512], W=[awT[i]])
                    pi = g % 2
                    for k in range(KC):
                        kb.mm(PS[pi][:, :], crep[:, k, :], awt[i][:, k, :], k == 0, False, [cTk, awT[i]], [PT[pi]])
                    kb.mm(PS[pi][:, :], ones_f[0:1, :], abt[i][:], False, True, [awT[i], cT], [PT[pi]])
                    kb.cp("act", modb[:, g * 512:(g + 1) * 512], PS[pi][:, :], [PT[pi]], [modT])
                kb.stt("dve", modb[:, D:2 * D], modb[:, D:2 * D], 1.0, n1gb[:], ALU.add, ALU.mult, [modT, cTk], [modT])
                kb.stt("dve", modb[:, 4 * D:5 * D], modb[:, 4 * D:5 * D], 1.0, n2gb[:], ALU.add, ALU.mult, [modT, cTk], [modT])
                if b == 0:
                    dbg_out("mod", modb[0:1, :], [1, 6 * D], [modT])
                kb.barrier()
            if stop == "M":
                break

            with ExitStack() as pb:
                hT = sb("hT", [128, KC, S], BF16, pb)
                hTT = [T() for _ in range(NCH)]
                WTT = sb("WTT", [32, S], F32, pb)
                WTTT = T()
                with ExitStack() as pa:
                    OFT = sb("OFT", [128, 4, S], BF16, pa)
                    ODT = sb("ODT", [128, 4, S], BF16, pa)
                    OFTT, ODTT = T(), T()
                    with ExitStack() as ph:
                        xt = [sb("xt%d" % i, [128, D], F32, ph) for i in range(2)]
                        xtT = [T(), T()]
                        junk = sb("junk", [128, D], BF16, ph)
                        jT = T()
                        st = sb("stat", [128, 4], F32, ph)
                        sT = T()
                        tmp = sb("tmp", [128, D], F32, ph)
                        tmpT = T()
                        hb = [sb("hb%d" % i, [128, D], BF16, ph) for i in range(2)]
                        hbT = [T(), T()]
                        for t in range(NT):
                            i = t % 2
                            kb.dma("sp", xt[i][:], L("x")[b, t * 128:(t + 1) * 128, :], W=[xtT[i]])
                            kb.act(junk[:], xt[i][:], AF.Square, [xtT[i]], [jT, sT], accum=st[:, 0:1])
                            kb.act(st[:, 1:2], st[:, 0:1], AF.Sqrt, [sT], [sT], bias=EPS, scale=1.0 / D)
                            kb.op("dve", lambda e: e.reciprocal(st[:, 2:3], st[:, 1:2]), [sT], [sT])
                            kb.stt("dve", tmp[:], xt[i][:], st[:, 2:3], modb[:, D:2 * D], ALU.mult, ALU.mult,
                                   [xtT[i], sT, modT], [tmpT])
                            kb.tt("pool", hb[i][:], tmp[:], modb[:, 0:D], ALU.add, [tmpT, modT], [hbT[i]])
                            for j in range(2):
                                pv = PS[6 + j][:, 0:256].bitcast(BF16).rearrange("p (a c) -> p a c", a=4)
                                for a in range(4):
                                    k = 4 * j + a
                                    kb.tr(pv[:, a, :], hb[i][:, k * 128:(k + 1) * 128], ident_b[:], [hbT[i], cT], [PT[6 + j]])
                                kb.cp("act" if j == 0 else "dve", hT[:, 4 * j:4 * j + 4, t * 128:(t + 1) * 128], pv,
                                      [PT[6 + j]], [hTT[t // 4]])
                        if b == 0:
                            dbg_out("hT", hT[:, 0, :], [128, S], hTT, BF16)
                        kb.barrier()
                    if stop == "N1":
                        break

                    with ExitStack() as ph:
                        FQT = sb("FQT", [70, 4, S], BF16, ph)
                        FKT = sb("FKT", [70, 4, S], BF16, ph)
                        FV = sb("FV", [128, NT, 512], BF16, ph)
                        G = sb("G", [8, S], F32, ph)
                        FQTT, FKTT, FVT, GT = T(), T(), T(), T()
                        with ExitStack() as p2:
                            wq = [sb("wq%d" % i, [128, KC, 512], BF16, p2) for i in range(1)]
                            wqT = [T()]
                            Gx = sb("Gx", [8, S], F32, p2)
                            wfl = sb("wfl", [128, KC, 8], BF16, p2)
                            kb.dma("pool", wq[0][:], win_v[:, :, C_FV:C_FV + 512], W=[wqT[0]])
                            for t in range(NT):
                                pi = t % 2
                                for k in range(KC):
                                    kb.mm(PS[pi][:, :], hT[:, k, t * 128:(t + 1) * 128], wq[0][:, k, :], k == 0, k == KC - 1,
                                          [wqT[0], hTT[t // 4]], [PT[pi]])
                                kb.cp("act" if pi == 0 else "dve", FV[:, t, :], PS[pi][:, :], [PT[pi]], [FVT])
                            kb.dma("pool", wfl[:], win_v[:, :, C_FL:C_FL + 8], W=[GT])
                            for c in range(NCH):
                                pi = 4 + c % 2
                                for k in range(KC):
                                    kb.mm(PS[pi][0:8, :], wfl[:, k, :], hT[:, k, c * 512:(c + 1) * 512], k == 0, k == KC - 1,
                                          [GT, hTT[c]], [PT[pi]])
                                kb.act(Gx[:, c * 512:(c + 1) * 512], PS[pi][0:8, :], AF.Exp, [PT[pi], cT], [GT], bias=nbfgt[:], scale=-1.0)
                            kb.act(Gx[:], Gx[:], AF.Ln, [GT], [GT], bias=1.0, scale=1.0)
                            kb.op("dve", lambda e: e.tensor_tensor_scan(G[:], Gx[:], Gx[:], 0.0, ALU.add, ALU.max), [GT], [GT])
                            if b == 0:
                                dbg_out("G", G[:], [8, S], [GT])
                            kb.barrier()
                        pbuf = [sb("pbuf%d" % i, [128, 512], BF16, ph) for i in range(3)]
                        pbT = [T() for _ in range(3)]
                        rinv = sb("rinv", [128, 512], F32, ph)
                        rT = T()
                        for hh in range(2):
                            with ExitStack() as p2:
                                wq = [sb("wq%d" % i, [128, KC, 256], BF16, p2) for i in range(2)]
                                wqT = [T(), T()]
                                sq = sb("sq", [64, 512], BF16, p2)
                                sqT = T()
                                rs = sb("rs", [64, 512], F32, p2)
                                rsT = T()
                                Gy = sb("Gy", [8, S], F32, p2)
                                Gs = [sb("Gs%d" % i, [8, S], BF16, p2) for i in range(2)]
                                GsT = T()
                                for qi, (cbase, dst, dstT, gcol) in enumerate(((C_FQ, FQT, FQTT, gq8[0:64, 0:1]), (C_FK, FKT, FKTT, gains[0:64, 1:2]))):
                                    kb.dma("pool", wq[qi][:], win_v[:, :, cbase + hh * 256:cbase + hh * 256 + 256], W=[wqT[qi]])
                                    for h in range(4):
                                        for c in range(NCH):
                                            pi = (h * NCH + c) % 2
                                            for k in range(KC):
                                                kb.mm(PS[pi][0:64, :], wq[qi][:, k, h * 64:(h + 1) * 64], hT[:, k, c * 512:(c + 1) * 512],
                                                      k == 0, k == KC - 1, [wqT[qi], hTT[c]], [PT[pi]])
                                            kb.act(sq[:], PS[pi][0:64, :], AF.Square, [PT[pi]], [sqT])
                                            kb.mm(PS[2 + pi][0:64, :], ones_b[0:64, :], sq[:], True, True, [sqT, cT], [PT[2 + pi]])
                                            kb.act(rs[:], PS[2 + pi][0:64, :], AF.Sqrt, [PT[2 + pi]], [rsT], bias=EPS, scale=1.0 / 64)
                                            kb.op("dve", lambda e: e.reciprocal(rs[:], rs[:]), [rsT], [rsT])
                                            kb.stt("dve", dst[0:64, h, c * 512:(c + 1) * 512], PS[pi][0:64, :], gcol, rs[:],
                                                   ALU.mult, ALU.mult, [PT[pi], rsT, cT], [dstT])
                                kb.memset("dve", FQT[64:70, :, :], 1.0, [FQTT])
                                kb.memset("dve", FKT[64:70, :, :], 1.0, [FKTT])
                                for j in range(3):
                                    src = G if j == 0 else Gy
                                    kb.cp("dve", Gs[0][:], src[:], [GT, GsT], [GsT])
                                    kb.ts("dve", Gs[1][:], Gs[0][:], -1.0, None, ALU.mult, None, [GsT], [GsT])
                                    for h in range(4):
                                        kb.dma("sp", FQT[64 + j:65 + j, h, :], Gs[1][4 * hh + h:4 * hh + h + 1, :], R=[GsT], W=[FQTT])
                                        kb.dma("sp", FKT[67 + j:68 + j, h, :], Gs[0][4 * hh + h:4 * hh + h + 1, :], R=[GsT], W=[FKTT])
                                    if j < 2:
                                        kb.tt("dve", Gy[:], src[:], Gs[0][:], ALU.subtract, [GT, GsT], [GsT])
                                if b == 0 and hh == 0:
                                    dbg_out("FQT0", FQT[:, 0, :], [70, S], [FQTT], BF16)
                                    dbg_out("FKT0", FKT[:, 0, :], [70, S], [FKTT], BF16)
                                kb.barrier()
                            osl = slice(64 * hh, 64 * hh + 64)
                            units = [(h, c, kt) for h in range(4) for c in range(NCH) for kt in range(4 * c + 4)]

                            def fox_s(ui):
                                h, c, kt = units[ui]
                                j = kt - 4 * c
                                c0 = 128 * j if j >= 0 else 0
                                ps = ui % 2
                                kb.mm(PS[ps][:, c0:512], FKT[0:70, h, kt * 128:(kt + 1) * 128],
                                      FQT[0:70, h, c * 512 + c0:(c + 1) * 512], True, j < 0, [FKTT, FQTT], [PT[ps]])
                                if j >= 0:
                                    kb.mm(PS[ps][:, c0:c0 + 128], trib[:], ident_b[:], False, True, [cT], [PT[ps]])
                                kb.act(pbuf[ui % 3][:, c0:512], PS[ps][:, c0:512], AF.Exp, [PT[ps]], [pbT[ui % 3]])

                            def fox_pv(ui):
                                h, c, kt = units[ui]
                                j = kt - 4 * c
                                c0 = 128 * j if j >= 0 else 0
                                nkt = 4 * c + 4
                                po = 2 + (h * NCH + c) % 2
                                pl = 4 + (h * NCH + c) % 2
                                hg = 4 * hh + h
                                pbi = ui % 3
                                kb.mm(PS[po][osl, c0:512], FV[:, kt, hg * 64:(hg + 1) * 64], pbuf[pbi][:, c0:512],
                                      kt == 0, kt == nkt - 1, [FVT, pbT[pbi]], [PT[po]])
                                kb.mm(PS[pl][osl, c0:512], ones_b[:, :], pbuf[pbi][:, c0:512],
                                      kt == 0, kt == nkt - 1, [cT, pbT[pbi]], [PT[pl]])
                                if kt == nkt - 1:
                                    kb.op("dve", lambda e: e.reciprocal(rinv[osl, :], PS[pl][osl, :]), [PT[pl]], [rT])
                                    kb.tt("dve", OFT[osl, h, c * 512:(c + 1) * 512], PS[po][osl, :], rinv[osl, :], ALU.mult,
                                          [PT[po], rT], [OFTT])

                            fox_s(0)
                            for ui in range(len(units)):
                                if ui + 1 < len(units):
                                    fox_s(ui + 1)
                                fox_pv(ui)
                            kb.barrier()
                        if b == 0:
                            dbg_out("OFT", OFT[:], [128, 4, S], [OFTT], BF16)
                        kb.barrier()
                    if stop == "A1":
                        break

                    with ExitStack() as ph:
                        DQT = sb("DQT", [128, 4, S], BF16, ph)
                        DK = [sb("DK%d" % i, [128, S], BF16, ph) for i in range(2)]
                        DV = sb("DV", [128, NT, 64], BF16, ph)
                        IQT = sb("IQT", [128, 4, S], BF16, ph)
                        IK = [sb("IK%d" % i, [128, S], BF16, ph) for i in range(2)]
                        IW = sb("IW", [128, NT, 8], F32, ph)
                        tabT, DQTT, DKTT, DVT, IQTT, IKTT, IWT = T(), T(), T(), T(), T(), T(), T()
                        with ExitStack() as p1:
                            CS = sb("CS", [128, S], F32, p1)
                            SN = sb("SN", [128, S], F32, p1)
                            with ExitStack() as p2:
                                posi = sb("posi", [128, S], I32, p2)
                                ra = sb("ra", [128, S], F32, p2)
                                ua = sb("ua", [128, S], F32, p2)
                                na = sb("na", [128, S], F32, p2)
                                kb.dma("sp", posi[:], L("pos")[b:b + 1, :].partition_broadcast(128), W=[tabT])
                                kb.cp("dve", ra[:], posi[:], [tabT], [tabT])
                                kb.ts("dve", ra[:], ra[:], gains[:, 6:7], 1.0 / TWO_PI, ALU.mult, ALU.mult, [tabT, cT], [tabT])
                                for which, dst in ((0, SN), (1, CS)):
                                    kb.ts("dve", ua[:], ra[:], 0.25 * which, None, ALU.add, None, [tabT], [tabT])
                                    kb.cp("dve", posi[:], ua[:], [tabT], [tabT])
                                    kb.cp("dve", na[:], posi[:], [tabT], [tabT])
                                    kb.tt("dve", ua[:], ua[:], na[:], ALU.subtract, [tabT], [tabT])
                                    kb.stt("dve", na[:], ua[:], 0.5, ua[:], ALU.is_gt, ALU.subtract, [tabT], [tabT])
                                    kb.stt("dve", ua[:], na[:], 0.5, na[:], ALU.is_gt, ALU.subtract, [tabT], [tabT])
                                    kb.act(dst[:], ua[:], AF.Sin, [tabT], [tabT], scale=TWO_PI * (1.0 - 1e-6))
                                kb.ts("dve", SN[:], SN[:], gains[:, 7:8], None, ALU.mult, None, [tabT, cT], [tabT])
                                if b == 0:
                                    dbg_out("CS", CS[0:64, :], [64, S], [tabT])
                                    dbg_out("SN", SN[0:64, :], [64, S], [tabT])
                                kb.barrier()
                            if stop == "T":
                                break
                            wsw_v = L("w_sw").rearrange("(k p) n -> p k n", p=128)
                            with ExitStack() as p2:
                                wA = sb("wA", [128, KC, 512], BF16, p2)
                                wB = sb("wB", [128, KC, 512], BF16, p2)
                                wA1 = sb("wA1", [128, KC, 64], BF16, p2)
                                wB1 = sb("wB1", [128, KC, 64], BF16, p2)
                                wiw = sb("wiw", [128, KC, 8], BF16, p2)
                                wT = T()
                                sq = sb("sq", [128, 512], BF16, p2)
                                rs = sb("rs", [128, 512], F32, p2)
                                t1 = sb("t1", [128, 512], F32, p2)
                                t2 = sb("t2", [128, 512], F32, p2)
                                t3 = sb("t3", [128, 512], BF16, p2)
                                sqT, rsT, t1T, t2T, t3T = T(), T(), T(), T(), T()
                                for i2 in range(2):
                                    kb.memset("pool", DK[i2][:], 0.0, [DKTT])
                                    kb.memset("pool", IK[i2][:], 0.0, [IKTT])

                                def rope_proj(cA, cB, nheads, dst3, dst2, dstT, gA, gB, norm):
                                    ncol = 64 * nheads
                                    wa, wb = (wA, wB) if nheads == 8 else (wA1, wB1)
                                    kb.dma("pool", wa[:, :, 0:ncol], win_v[:, :, cA:cA + ncol], W=[wT])
                                    kb.dma("pool", wb[:, :, 0:ncol], wsw_v[:, :, cB:cB + ncol], W=[wT])
                                    pairs = [(h, h + 4) for h in range(4)] if nheads == 8 else [(0, 0)]
                                    for (hl, hu) in pairs:
                                        for c in range(NCH):
                                            pi = c % 2
                                            csl = slice(c * 512, (c + 1) * 512)
                                            for (hh_, p0) in ((hl, 0), (hu, 64)):
                                                osl = slice(p0, p0 + 64)
                                                for k in range(KC):
                                                    kb.mm(PS[pi][osl, :], wa[:, k, hh_ * 64:(hh_ + 1) * 64], hT[:, k, csl], k == 0, k == KC - 1,
                                                          [wT, hTT[c]], [PT[pi]])
                                                for k in range(KC):
                                                    kb.mm(PS[2 + pi][osl, :], wb[:, k, hh_ * 64:(hh_ + 1) * 64], hT[:, k, csl], k == 0, k == KC - 1,
                                                          [wT, hTT[c]], [PT[2 + pi]])
                                            if norm:
                                                kb.act(sq[:], PS[pi][:, :], AF.Square, [PT[pi]], [sqT])
                                                kb.mm(PS[4 + pi][:, :], bd_ones[:], sq[:], True, True, [sqT, cT], [PT[4 + pi]])
                                                kb.act(rs[:], PS[4 + pi][:, :], AF.Sqrt, [PT[4 + pi]], [rsT], bias=EPS, scale=1.0 / 64)
                                                kb.op("dve", lambda e: e.reciprocal(rs[:], rs[:]), [rsT], [rsT])
                                                kb.stt("dve", t1[:], PS[pi][:, :], gA, rs[:], ALU.mult, ALU.mult, [PT[pi], rsT, cT], [t1T])
                                                kb.stt("dve", t2[:], PS[2 + pi][:, :], gB, rs[:], ALU.mult, ALU.mult, [PT[2 + pi], rsT, cT], [t2T])
                                                kb.tt("pool", t1[:], t1[:], CS[:, csl], ALU.mult, [t1T, tabT], [t1T])
                                                kb.tt("pool", t2[:], t2[:], SN[:, csl], ALU.mult, [t2T, tabT], [t2T])
                                            else:
                                                kb.tt("dve", t1[:], PS[pi][:, :], CS[:, csl], ALU.mult, [PT[pi], tabT], [t1T])
                                                kb.tt("dve", t2[:], PS[2 + pi][:, :], SN[:, csl], ALU.mult, [PT[2 + pi], tabT], [t2T])
                                            if dst3 is not None:
                                                kb.tt("pool", dst3[:, hl, csl], t1[:], t2[:], ALU.add, [t1T, t2T], [dstT])
                                            else:
                                                kb.tt("pool", t3[:], t1[:], t2[:], ALU.add, [t1T, t2T], [t3T])
                                                kb.cp("dve", dst2[0][0:64, csl], t3[0:64, :], [t3T], [dstT])
                                                kb.cp("dve", dst2[1][64:128, csl], t3[64:128, :], [t3T], [dstT])

                                rope_proj(C_DQ, 0, 8, DQT, None, DQTT, gq8[:, 1:2], gq8[:, 2:3], True)
                                rope_proj(C_DK, 512, 1, None, DK, DKTT, gains[:, 3:4], gains[:, 5:6], True)
                                rope_proj(C_IQ, 576, 8, IQT, None, IQTT, None, None, False)
                                rope_proj(C_IK, 1088, 1, None, IK, IKTT, None, None, False)
                                kb.dma("pool", wA1[:], win_v[:, :, C_DV:C_DV + 64], W=[wT])
                                kb.dma("pool", wiw[:], win_v[:, :, C_IW:C_IW + 8], W=[wT])
                                for t in range(NT):
                                    pi = t % 2
                                    for k in range(KC):
                                        kb.mm(PS[pi][:, 0:64], hT[:, k, t * 128:(t + 1) * 128], wA1[:, k, :], k == 0, k == KC - 1,
                                              [wT, hTT[t // 4]], [PT[pi]])
                                    for k in range(KC):
                                        kb.mm(PS[2 + pi][:, 0:8], hT[:, k, t * 128:(t + 1) * 128], wiw[:, k, :], k == 0, k == KC - 1,
                                              [wT, hTT[t // 4]], [PT[2 + pi]])
                                    kb.cp("act", DV[:, t, :], PS[pi][:, 0:64], [PT[pi]], [DVT])
                                    kb.cp("dve", IW[:, t, :], PS[2 + pi][:, 0:8], [PT[2 + pi]], [IWT])
                                if b == 0:
                                    dbg_out("DQT", DQT[:], [128, 4, S], [DQTT], BF16)
                                    dbg_out("DKT", DK[0][:], [128, S], [DKTT], BF16)
                                    dbg_out("IQT", IQT[:], [128, 4, S], [IQTT], BF16)
                                    dbg_out("IKT", IK[1][:], [128, S], [IKTT], BF16)
                                    dbg_out("IW", IW[:], [128, NT, 8], [IWT])
                                kb.barrier()
                        if stop in ("P2", "T"):
                            break
                        scb = [sb("scb%d" % i, [128, S], F32, ph) for i in range(2)]
                        scT = [T(), T()]
                        MB = [sb("MB%d" % i, [128, S], BF16, ph) for i in range(2)]
                        MBT = [T(), T()]
                        junk = sb("junkb", [128, S], BF16, ph)
                        jT = T()
                        rbuf = [sb("rbuf%d" % i, [128, 512], F32, ph) for i in range(2)]
                        rbT = [T(), T()]
                        bs = [sb("bs%d" % i, [128, 8], F32, ph) for i in range(2)]
                        bsT = [T(), T()]
                        pbuf = [sb("pbufd%d" % i, [128, 512], BF16, ph) for i in range(3)]
                        pbT = [T() for _ in range(3)]
                        rinv = sb("rinvd", [128, 512], F32, ph)
                        rT = T()
                        ctr = {"u": 0, "ur": 0}

                        def dsa_index(i):
                            end = 128 * (i + 1)
                            sc = scb[i % 2]
                            sT_ = scT[i % 2]
                            qsl = slice(i * 128, (i + 1) * 128)
                            ng = (end + 511) // 512
                            for h in range(8):
                                for g in range(ng):
                                    n = min(512, end - 512 * g)
                                    ps = ctr["ur"] % 2
                                    ri = ctr["ur"] % 2
                                    ctr["ur"] += 1
                                    kb.mm(PS[ps][:, 0:n], IQT[:, h % 4, qsl], IK[h // 4][:, g * 512:g * 512 + n], True, True,
                                          [IQTT, IKTT], [PT[ps]])
                                    kb.act(rbuf[ri][:, 0:n], PS[ps][:, 0:n], AF.Relu, [PT[ps]], [rbT[ri]])
                                    if h == 0:
                                        kb.ts("dve", sc[:, g * 512:g * 512 + n], rbuf[ri][:, 0:n], IW[:, i, 0:1], None, ALU.mult, None,
                                              [rbT[ri], IWT], [sT_])
                                    else:
                                        kb.stt("dve", sc[:, g * 512:g * 512 + n], rbuf[ri][:, 0:n], IW[:, i, h:h + 1],
                                               sc[:, g * 512:g * 512 + n], ALU.mult, ALU.add, [rbT[ri], IWT, sT_], [sT_])
                            bsx = bs[i % 2]
                            bsT_ = bsT[i % 2]
                            if i >= 2:
                                kb.op("dve", lambda e: e.tensor_reduce(bsx[:, 0:1], sc[:, 0:end], AX.X, ALU.max), [sT_], [bsT_])
                                kb.op("dve", lambda e: e.tensor_reduce(bsx[:, 1:2], sc[:, 0:end], AX.X, ALU.min), [sT_], [bsT_])
                                kb.tt("dve", bsx[:, 2:3], bsx[:, 0:1], bsx[:, 1:2], ALU.subtract, [bsT_], [bsT_])
                                kb.memset("dve", sc[0:64, end - 64:end], -1e30, [sT_])
                                for it in range(N_BISECT):
                                    f = 2.0 ** (-(it + 1))
                                    kb.stt("dve", bsx[:, 3:4], bsx[:, 2:3], f, bsx[:, 1:2], ALU.mult, ALU.add, [bsT_], [bsT_])
                                    kb.ts("dve", junk[:, 0:end], sc[:, 0:end], bsx[:, 3:4], None, ALU.is_ge, ALU.add, [sT_, bsT_], [jT, bsT_],
                                          accum=bsx[:, 4:5])
                                    kb.ts("dve", bsx[:, 5:6], bsx[:, 4:5], 256.0, f, ALU.is_ge, ALU.mult, [bsT_], [bsT_])
                                    kb.stt("dve", bsx[:, 1:2], bsx[:, 5:6], bsx[:, 2:3], bsx[:, 1:2], ALU.mult, ALU.add, [bsT_], [bsT_])
                            else:
                                kb.memset("dve", bsx[:, 1:2], -1e29, [bsT_])
                                kb.memset("dve", sc[0:64, end - 64:end], -1e30, [sT_])
                            kb.ts("dve", MB[i % 2][:, 0:end], sc[:, 0:end], bsx[:, 1:2], NEG, ALU.is_lt, ALU.mult, [sT_, bsT_], [MBT[i % 2]])

                        def dsa_attn(i):
                            qsl = slice(i * 128, (i + 1) * 128)
                            mb = MB[i % 2]
                            mT_ = MBT[i % 2]
                            aunits = [(hg, kt) for hg in range(2) for kt in range(i + 1)]

                            def a_s(ai):
                                hg, kt = aunits[ai]
                                gu = ctr["u"] + ai
                                ps = 6 + gu % 2
                                ksl = slice(kt * 128, (kt + 1) * 128)
                                psv = PS[ps][:, :].rearrange("p (a c) -> p a c", a=4)
                                kb.mm(psv, DK[hg][:, ksl], DQT[:, :, qsl], True, False, [DKTT, DQTT], [PT[ps]])
                                kb.mm(psv, mb[:, ksl], ident4[:], False, True, [mT_, cT], [PT[ps]])
                                kb.act(pbuf[gu % 3][:], PS[ps][:, :], AF.Exp, [PT[ps]], [pbT[gu % 3]])

                            def a_pv(ai):
                                hg, kt = aunits[ai]
                                gu = ctr["u"] + ai
                                hs = slice(64 * hg, 64 * hg + 64)
                                po = 2 + (2 * i + hg) % 2
                                pl = 4 + (2 * i + hg) % 2
                                pbi = gu % 3
                                kb.mm(PS[po][hs, :], DV[:, kt, :], pbuf[pbi][:], kt == 0, kt == i, [DVT, pbT[pbi]], [PT[po]])
                                kb.mm(PS[pl][hs, :], ones_b[:, :], pbuf[pbi][:], kt == 0, kt == i, [cT, pbT[pbi]], [PT[pl]])
                                if kt == i:
                                    kb.op("dve", lambda e: e.reciprocal(rinv[hs, :], PS[pl][hs, :]), [PT[pl]], [rT])
                                    kb.tt("dve", ODT[hs, :, qsl], PS[po][hs, :].rearrange("p (a c) -> p a c", a=4),
                                          rinv[hs, :].rearrange("p (a c) -> p a c", a=4), ALU.mult, [PT[po], rT], [ODTT])

                            a_s(0)
                            for ai in range(len(aunits)):
                                if ai + 1 < len(aunits):
                                    a_s(ai + 1)
                                a_pv(ai)
                            ctr["u"] += len(aunits)

                        dsa_index(0)
                        for i in range(NT):
                            if i + 1 < NT:
                                dsa_index(i + 1)
                            dsa_attn(i)
                        if b == 0:
                            dbg_out("ODT", ODT[:], [128, 4, S], [ODTT], BF16)
                        kb.barrier()
                    if stop == "A2":
                        break

                    with ExitStack() as ph:
                        MT = sb("MT", [128, KC, S], BF16, ph)
                        MTT = [T() for _ in range(NCH)]
                        with ExitStack() as p2:
                            wpf = sb("wpf", [128, 4, D], BF16, p2)
                            wpd = sb("wpd", [128, 4, D], BF16, p2)
                            wstg = sb("wstg", [128, 4, D], F32, p2)
                            wpT = T()
                            wsT = T()
                            for nm, dstw in (("w_proj_fox", wpf), ("w_proj_dsa", wpd)):
                                wv = L(nm).rearrange("(a q d) n -> a d q n", a=2, q=4)
                                for a in range(2):
                                    kb.dma("sp", wstg[64 * a:64 * a + 64, :, :], wv[a], W=[wsT])
                                kb.cp("pool", dstw[:], wstg[:], [wsT], [wpT])
                            gwf = [sb("gwf%d" % i, [128, KC, 128], BF16, p2) for i in range(2)]
                            gwd = [sb("gwd%d" % i, [128, KC, 128], BF16, p2) for i in range(2)]
                            gwT = [T(), T()]
                            sf = [sb("sf%d" % i, [128, 512], F32, p2) for i in range(2)]
                            sd = [sb("sd%d" % i, [128, 512], F32, p2) for i in range(2)]
                            sfT = [T(), T()]
                            sdT = [T(), T()]
                            def g_load(cc_):
                                i_ = cc_ % 2
                                kb.dma("pool", gwf[i_][:], win_v[:, :, C_GF + cc_ * 128:C_GF + (cc_ + 1) * 128], W=[gwT[i_]])
                                kb.dma("pool", gwd[i_][:], win_v[:, :, C_GD + cc_ * 128:C_GD + (cc_ + 1) * 128], W=[gwT[i_]])

                            g_load(0)
                            for cc in range(KC):
                                i = cc % 2
                                if cc + 1 < KC:
                                    g_load(cc + 1)
                                for c in range(NCH):
                                    csl = slice(c * 512, (c + 1) * 512)
                                    par = (cc * NCH + c) % 2
                                    b0 = 4 * par
                                    for q in range(4):
                                        kb.mm(PS[b0][:, :], wpf[:, q, cc * 128:(cc + 1) * 128], OFT[:, q, csl], q == 0, q == 3,
                                              [wpT, OFTT], [PT[b0]])
                                    for q in range(4):
                                        kb.mm(PS[b0 + 1][:, :], wpd[:, q, cc * 128:(cc + 1) * 128], ODT[:, q, csl], q == 0, q == 3,
                                              [wpT, ODTT], [PT[b0 + 1]])
                                    for k in range(KC):
                                        kb.mm(PS[b0 + 2][:, :], gwf[i][:, k, :], hT[:, k, csl], k == 0, k == KC - 1,
                                              [gwT[i], hTT[c]], [PT[b0 + 2]])
                                    for k in range(KC):
                                        kb.mm(PS[b0 + 3][:, :], gwd[i][:, k, :], hT[:, k, csl], k == 0, k == KC - 1,
                                              [gwT[i], hTT[c]], [PT[b0 + 3]])
                                    kb.act(sf[par][:], PS[b0 + 2][:, :], AF.Sigmoid, [PT[b0 + 2], cT], [sfT[par]], bias=bgate[:, cc:cc + 1])
                                    kb.act(sd[par][:], PS[b0 + 3][:, :], AF.Sigmoid, [PT[b0 + 3], cT], [sdT[par]], bias=bgate[:, 8 + cc:9 + cc])
                                    kb.tt("dve", sf[par][:], sf[par][:], PS[b0][:, :], ALU.mult, [sfT[par], PT[b0]], [sfT[par]])
                                    kb.tt("dve", sd[par][:], sd[par][:], PS[b0 + 1][:, :], ALU.mult, [sdT[par], PT[b0 + 1]], [sdT[par]])
                                    kb.tt("pool", MT[:, cc, csl], sf[par][:], sd[par][:], ALU.add, [sfT[par], sdT[par]], [MTT[c]])
                            if b == 0:
                                dbg_out("MT", MT[:, 0, :], [128, S], MTT, BF16)
                            kb.barrier()
                        if stop == "G":
                            break
                        with ExitStack() as p2:
                            wo_b = sb("wo_b", [128, KC, D], BF16, p2)
                            woT = T()
                            kb.dma("pool", wo_b[:], L("w_out").rearrange("(k p) n -> p k n", p=128), W=[woT])
                            xt = [sb("xo%d" % i, [128, D], F32, p2) for i in range(2)]
                            xtT = [T(), T()]
                            tmp = sb("tmpo", [128, D], F32, p2)
                            tmpT = T()
                            junk = sb("junko", [128, D], BF16, p2)
                            jT = T()
                            h2f = sb("h2f", [128, D], F32, p2)
                            h2fT = T()
                            h2hi = sb("h2hi", [128, D], BF16, p2)
                            h2lo = sb("h2lo", [128, D], BF16, p2)
                            loTt = sb("loTt", [128, KC, 128], BF16, p2)
                            hiT, loT, loTT = T(), T(), T()
                            st = sb("stato", [128, 4], F32, p2)
                            sT = T()
                            rt = sb("rt", [128, 160], F32, p2)
                            rtT = T()
                            for t in range(NT):
                                i = t % 2
                                tsl = slice(t * 128, (t + 1) * 128)
                                for half in range(2):
                                    for cc in range(KC):
                                        kb.mm(PS[half][:, :], MT[:, cc, tsl], wo_b[:, cc, half * 512:(half + 1) * 512], cc == 0, cc == KC - 1,
                                              [MTT[t // 4], woT], [PT[half]])
                                kb.dma("sp", xt[i][:], L("x")[b, tsl, :], W=[xtT[i]])
                                for half in range(2):
                                    hsl = slice(half * 512, (half + 1) * 512)
                                    kb.tt("dve", tmp[:, hsl], PS[half][:, :], modb[:, 2 * D + half * 512:2 * D + (half + 1) * 512], ALU.mult,
                                          [PT[half], modT], [tmpT])
                                kb.tt("pool", xt[i][:], xt[i][:], tmp[:], ALU.add, [xtT[i], tmpT], [xtT[i]])
                                kb.dma("sp", y_d[b, tsl, :], xt[i][:], R=[xtT[i]], W=[yT[b][t]])
                                if stop == "O1":
                                    continue
                                kb.act(junk[:], xt[i][:], AF.Square, [xtT[i]], [jT, sT], accum=st[:, 0:1])
                                kb.act(st[:, 1:2], st[:, 0:1], AF.Sqrt, [sT], [sT], bias=EPS, scale=1.0 / D)
                                kb.op("dve", lambda e: e.reciprocal(st[:, 2:3], st[:, 1:2]), [sT], [sT])
                                kb.stt("dve", tmp[:], xt[i][:], st[:, 2:3], modb[:, 4 * D:5 * D], ALU.mult, ALU.mult,
                                       [xtT[i], sT, modT], [tmpT])
                                kb.tt("pool", h2f[:], tmp[:], modb[:, 3 * D:4 * D], ALU.add, [tmpT, modT], [h2fT])
                                kb.cp("dve", h2hi[:], h2f[:], [h2fT], [hiT])
                                kb.tt("pool", h2lo[:], h2f[:], h2hi[:], ALU.subtract, [h2fT, hiT], [loT])
                                for j in range(2):
                                    pv = PS[2 + j][:, 0:256].bitcast(BF16).rearrange("p (a c) -> p a c", a=4)
                                    for a in range(4):
                                        k = 4 * j + a
                                        kb.tr(pv[:, a, :], h2hi[:, k * 128:(k + 1) * 128], ident_b[:], [hiT, cT], [PT[2 + j]])
                                    kb.cp("act" if j == 0 else "dve", hT[:, 4 * j:4 * j + 4, tsl], pv, [PT[2 + j]], [hTT[t // 4]])
                                for j in range(2):
                                    pv = PS[6 + j][:, 0:256].bitcast(BF16).rearrange("p (a c) -> p a c", a=4)
                                    for a in range(4):
                                        k = 4 * j + a
                                        kb.tr(pv[:, a, :], h2lo[:, k * 128:(k + 1) * 128], ident_b[:], [loT, cT], [PT[6 + j]])
                                    kb.cp("act" if j == 0 else "dve", loTt[:, 4 * j:4 * j + 4, :], pv, [PT[6 + j]], [loTT])
                                if stop == "O2":
                                    continue
                                nmm = 3 * KC
                                q = 0
                                for (lh, lhT_, wgt) in ((hT, hTT[t // 4], wr_hi), (hT, hTT[t // 4], wr_lo), (loTt, loTT, wr_hi)):
                                    for k in range(KC):
                                        lhs_ap = lh[:, k, tsl] if lh is hT else lh[:, k, :]
                                        kb.mm(PS[4][:, 0:36], lhs_ap, wgt[:, k, :], q == 0, q == nmm - 1, [lhT_, cT], [PT[4]])
                                        q += 1
                                if stop == "O3":
                                    continue
                                R_ = [rtT]
                                lg = rt[:, 0:36]
                                kb.tt("dve", lg, PS[4][:, 0:36], brb[:], ALU.add, [PT[4], cT], R_)
                                kb.op("dve", lambda e: e.tensor_reduce(rt[:, 40:41], rt[:, 0:4], AX.X, ALU.max), R_, R_)
                                kb.ts("dve", rt[:, 44:48], rt[:, 0:4], rt[:, 40:41], None, ALU.is_ge, None, R_, R_)
                                kb.ts("dve", rt[:, 41:42], rt[:, 40:41], -1.0, None, ALU.mult, None, R_, R_)
                                kb.act(rt[:, 48:52], rt[:, 0:4], AF.Exp, R_, R_, bias=rt[:, 41:42], accum=rt[:, 42:43])
                                kb.op("dve", lambda e: e.reciprocal(rt[:, 43:44], rt[:, 42:43]), R_, R_)
                                kb.ts("dve", rt[:, 52:56], rt[:, 44:48], -1.0, 1e30, ALU.add, ALU.mult, R_, R_)
                                for gi in range(4):
                                    kb.ts("dve", rt[:, 60 + 8 * gi:68 + 8 * gi], rt[:, 4 + 8 * gi:12 + 8 * gi], rt[:, 44 + gi:45 + gi],
                                          rt[:, 52 + gi:53 + gi], ALU.mult, ALU.add, R_, R_)
                                lem = rt[:, 60:92]
                                kb.op("dve", lambda e: e.tensor_reduce(rt[:, 56:57], rt[:, 60:92], AX.X, ALU.max), R_, R_)
                                kb.ts("dve", rt[:, 92:124], lem, rt[:, 56:57], None, ALU.is_ge, None, R_, R_)
                                kb.stt("dve", lem, rt[:, 92:124], -1e30, lem, ALU.mult, ALU.add, R_, R_)
                                kb.op("dve", lambda e: e.tensor_reduce(rt[:, 57:58], rt[:, 60:92], AX.X, ALU.max), R_, R_)
                                kb.ts("dve", rt[:, 124:156], lem, rt[:, 57:58], None, ALU.is_ge, None, R_, R_)
                                kb.tt("dve", rt[:, 58:59], rt[:, 57:58], rt[:, 56:57], ALU.subtract, R_, R_)
                                kb.act(rt[:, 59:60], rt[:, 58:59], AF.Exp, R_, R_)
                                kb.ts("dve", rt[:, 36:37], rt[:, 59:60], 1.0, None, ALU.add, None, R_, R_)
                                kb.op("dve", lambda e: e.reciprocal(rt[:, 36:37], rt[:, 36:37]), R_, R_)
                                kb.tt("dve", rt[:, 37:38], rt[:, 36:37], rt[:, 43:44], ALU.mult, R_, R_)
                                kb.tt("dve", rt[:, 38:39], rt[:, 37:38], rt[:, 59:60], ALU.mult, R_, R_)
                                kb.ts("dve", rt[:, 92:124], rt[:, 92:124], rt[:, 37:38], None, ALU.mult, None, R_, R_)
                                kb.stt("dve", rt[:, 92:124], rt[:, 124:156], rt[:, 38:39], rt[:, 92:124], ALU.mult, ALU.add, R_, R_)
                                if stop == "O4":
                                    continue
                                kb.mm(PS[5][0:32, 0:128], rt[:, 92:124], ident_f[:], True, True, [rtT, cT], [PT[5]])
                                kb.cp("act", WTT[:, tsl], PS[5][0:32, 0:128], [PT[5]], [WTTT])
                            if b == 0:
                                dbg_out("WTT", WTT[:], [32, S], [WTTT])
                                dbg_out("h2T", hT[:, 0, :], [128, S], hTT, BF16)
                            kb.barrier()
                if stop in ("O", "O1", "O2", "O3", "O4"):
                    break
                with ExitStack() as ph:
                    XM = sb("XM", [128, NT, D], F32, ph)
                    XMT = [T() for _ in range(NT)]
                    W1 = [sb("W1_%d" % i, [128, KC, 512], BF16, ph) for i in range(2)]
                    W3 = [sb("W3_%d" % i, [128, KC, 512], BF16, ph) for i in range(2)]
                    W2 = [sb("W2_%d" % i, [128, 4, D], BF16, ph) for i in range(2)]
                    wT = [T(), T()]
                    w2T = [T(), T()]
                    gT = [sb("gT%d" % i, [128, 4, 512], BF16, ph) for i in range(2)]
                    gTT = [T(), T()]
                    sa = [sb("sa%d" % i, [128, 512], F32, ph) for i in range(2)]
                    saT = [T(), T()]
                    g1 = [sb("g1%d" % i, [128, 512], BF16, ph) for i in range(2)]
                    g1T = [T(), T()]
                    wtb = [sb("wtb%d" % i, [128, 512], F32, ph) for i in range(2)]
                    wtbT = [T(), T()]
                    selE = [sb("selE%d" % i, [32, 128], F32, ph) for i in range(2)]
                    selT = [T(), T()]
                    xo = sb("xfin", [128, D], F32, ph)
                    xoT = T()
                    ne_run = NE if stop != "E1" else 2

                    def moe_load(e):
                        i = e % 2
                        kb.dma("pool", W1[i][:], L("exp_w1")[e].rearrange("(k p) f -> p k f", p=128), W=[wT[i]])
                        kb.dma("pool", W3[i][:], L("exp_w3")[e].rearrange("(k p) f -> p k f", p=128), W=[wT[i]])
                        kb.dma("pool", W2[i][:], L("exp_w2")[e].rearrange("(k p) n -> p k n", p=128), W=[w2T[i]])

                    def moe_scale(e):
                        i = e % 2
                        for fc in range(4):
                            kb.tt("pool", W2[i][:, fc, :], W2[i][:, fc, :], modb[:, 5 * D:6 * D], ALU.mult, [w2T[i], modT], [w2T[i]])
                        kb.cp("dve", selE[i][:], ident_f[0:32, e:e + 1].to_broadcast([32, 128]), [cT], [selT[i]])

                    munits = [(e, c) for e in range(ne_run) for c in range(NCH)]

                    def moe_ab(ui):
                        e, c = munits[ui]
                        i = e % 2
                        csl = slice(c * 512, (c + 1) * 512)
                        par = ui % 2
                        kb.mm(PS[6][:, :], selE[i][:], WTT[:, csl], True, True, [selT[i], WTTT], [PT[6]])
                        kb.cp("act", wtb[par][:], PS[6][:, :], [PT[6]], [wtbT[par]])
                        for fc in range(4):
                            q = fc % 2
                            fsl = slice(fc * 128, (fc + 1) * 128)
                            for k in range(KC):
                                kb.mm(PS[2 * q][:, :], W1[i][:, k, fsl], hT[:, k, csl], k == 0, k == KC - 1, [wT[i], hTT[c]], [PT[2 * q]])
                            for k in range(KC):
                                kb.mm(PS[2 * q + 1][:, :], W3[i][:, k, fsl], hT[:, k, csl], k == 0, k == KC - 1, [wT[i], hTT[c]], [PT[2 * q + 1]])
                            kb.act(sa[q][:], PS[2 * q][:, :], AF.Silu, [PT[2 * q]], [saT[q]])
                            kb.tt("dve", g1[q][:], sa[q][:], PS[2 * q + 1][:, :], ALU.mult, [saT[q], PT[2 * q + 1]], [g1T[q]])
                            kb.tt("pool", gT[par][:, fc, :], g1[q][:], wtb[par][:], ALU.mult, [g1T[q], wtbT[par]], [gTT[par]])

                    def moe_y(ui):
                        e, c = munits[ui]
                        i = e % 2
                        par = ui % 2
                        for tq in range(4):
                            t = 4 * c + tq
                            for half in range(2):
                                py = 4 + (2 * tq + half) % 2
                                hsl = slice(half * 512, (half + 1) * 512)
                                for fc in range(4):
                                    kb.mm(PS[py][:, :], gT[par][:, fc, tq * 128:(tq + 1) * 128], W2[i][:, fc, hsl], fc == 0, fc == 3,
                                          [gTT[par], w2T[i]], [PT[py]])
                                if e == 0:
                                    kb.cp("dve", XM[:, t, hsl], PS[py][:, :], [PT[py]], [XMT[t]])
                                else:
                                    kb.tt("dve", XM[:, t, hsl], XM[:, t, hsl], PS[py][:, :], ALU.add, [XMT[t], PT[py]], [XMT[t]])

                    moe_load(0)
                    moe_scale(0)
                    for ui in range(len(munits)):
                        e, c = munits[ui]
                        moe_ab(ui)
                        if ui >= 1:
                            moe_y(ui - 1)
                        if c == 0 and e + 1 < ne_run:
                            moe_load(e + 1)
                        if c == NCH - 1 and e + 1 < ne_run:
                            moe_scale(e + 1)
                    moe_y(len(munits) - 1)
                    for t in range(NT):
                        tsl = slice(t * 128, (t + 1) * 128)
                        kb.dma("sp", xo[:], y_d[b, tsl, :], R=[yT[b][t]], W=[xoT])
                        kb.tt("pool", xo[:], xo[:], XM[:, t, :], ALU.add, [xoT, XMT[t]], [xoT])
                        kb.dma("sp", y_d[b, tsl, :], xo[:], R=[xoT], W=[yT[b][t]])
                    kb.barrier()
        kb.barrier(("sp",))
    return nc, (dbg_d, set(_L.got))


def _consts():
    ident = np.eye(128, dtype=np.float32)
    q = np.arange(128)[:, None]
    k = np.arange(128)[None, :]
    trib = np.where(k > q, NEG, 0.0).astype(np.float32)
    sel = np.zeros((32, NE, 128), np.float32)
    for e in range(NE):
        sel[e, e, :] = 1.0
    half = 32
    inv = (np.float32(10000.0) ** (-np.arange(half, dtype=np.float32) / np.float32(half))).astype(np.float32)
    invf = np.concatenate([inv, inv]).astype(np.float32)
    sgn = np.concatenate([-np.ones(32, np.float32), np.ones(32, np.float32)])
    return ident, trib, sel.reshape(32, NE * 128), invf, sgn


def _swap_cols(w, base, nheads):
    cols = []
    for h in range(nheads):
        cols.append(w[:, base + h * 64 + 32: base + h * 64 + 64])
        cols.append(w[:, base + h * 64: base + h * 64 + 32])
    return np.concatenate(cols, axis=1)


def prep_shared(inp):
    f = lambda a: np.ascontiguousarray(np.asarray(a, dtype=np.float32))
    ident, trib, sel, invf, sgn = _consts()
    w_in = f(inp["w_in"][0])
    w_sw = np.concatenate([_swap_cols(w_in, C_DQ, 8), _swap_cols(w_in, C_DK, 1),
                           _swap_cols(w_in, C_IQ, 8), _swap_cols(w_in, C_IK, 1)], axis=1)
    sw = lambda g: np.concatenate([g[32:], g[:32]])
    qd, kd = f(inp["qn_dsa"][0]), f(inp["kn_dsa"][0])
    gains = np.stack([f(inp["qn_fox"][0]), f(inp["kn_fox"][0]), qd, kd, sw(qd), sw(kd), invf, sgn], axis=1)
    sh = {
        "ada_w": f(inp["ada_w"][0]), "ada_b": f(inp["ada_b"][0]).reshape(1, -1),
        "norm1_g": f(inp["norm1_g"][0]).reshape(1, -1), "norm2_g": f(inp["norm2_g"][0]).reshape(1, -1),
        "w_in": w_in, "w_sw": f(w_sw), "b_fgt": f(inp["b_fgt"][0]).reshape(8, 1),
        "b_gate": f(f(inp["b_gate"][0]).reshape(16, 128).T), "gains": f(gains),
        "w_proj_fox": f(inp["w_proj_fox"][0]), "w_proj_dsa": f(inp["w_proj_dsa"][0]), "w_out": f(inp["w_out"][0]),
        "w_router": f(np.concatenate([inp["router_w_grp"][0], inp["router_w_exp"][0]], axis=1)),
        "b_router": f(np.concatenate([inp["router_b_grp"][0], inp["router_b_exp"][0]])).reshape(1, 36),
        "exp_w1": f(inp["exp_w1"][0]), "exp_w3": f(inp["exp_w3"][0]), "exp_w2": f(inp["exp_w2"][0]),
        "ident": ident, "trib": trib, "sel": sel,
    }
    return sh


def prep_core(inp, b0, nb):
    x = np.ascontiguousarray(np.asarray(inp["x"][b0:b0 + nb], dtype=np.float32))
    c = np.asarray(inp["c"][b0:b0 + nb], dtype=np.float32)
    cT = np.ascontiguousarray(c.reshape(nb, KC, 128).transpose(0, 2, 1))
    pos = np.ascontiguousarray(np.asarray(inp["positions"][b0:b0 + nb], dtype=np.int32))
    return {"x": x, "cT": cT, "pos": pos}


def kernel(**inputs):
    n = 8
    nb = 4
    nc, (_, used) = build(nb)
    sh = prep_shared(inputs)
    in_maps = []
    for i in range(n):
        m = dict(sh)
        m.update(prep_core(inputs, i * nb, nb))
        in_maps.append({k: v for k, v in m.items() if k in used})
    res = run_bass_kernel_spmd(nc, in_maps, core_ids=list(range(n)))
    return np.concatenate([r["y"] for r in res.results], axis=0).astype(np.float32)
```
